# Optimizing a Trainium2 kernel written in Bass

```python
import math
import jax
import jax.numpy as jnp
from jax import lax
import numpy as np

D_MODEL = 1024
BATCH = 4
SEQ = 4096
DEPTH = 1

CHUNK = 64
Q_BLOCK = 128
EPS = 1e-6
NEG_INF = -1e30

N_ATTN_HEADS = 8
HEAD_DIM = D_MODEL // (2 * N_ATTN_HEADS)
V_HEAD_DIM = 2 * HEAD_DIM
QK_WIDTH = N_ATTN_HEADS * 2 * HEAD_DIM
ATTN_WIDTH = N_ATTN_HEADS * V_HEAD_DIM

D_RNN = (5 * D_MODEL) // 4
RNN_BLOCK = 64
N_RNN_BLOCKS = D_RNN // RNN_BLOCK
CONV_WIDTH = 4
RG_C = 8.0

N_BRANCH = 2
IN_SPLITS = (QK_WIDTH, 2 * QK_WIDTH, 2 * QK_WIDTH + ATTN_WIDTH,
             2 * QK_WIDTH + ATTN_WIDTH + D_RNN, 2 * QK_WIDTH + ATTN_WIDTH + 2 * D_RNN)
IN_COLS = 2 * QK_WIDTH + ATTN_WIDTH + 2 * D_RNN + N_BRANCH * D_MODEL

PEER_HEADS = 8
PEER_N_KEYS = 128
PEER_N_EXPERTS = PEER_N_KEYS ** 2
PEER_QDIM = 256
PEER_HALF = PEER_QDIM // 2
PEER_TOPK = 16
PEER_BLOCK = 128

kernel_name = 'hybrid_diffattn_rglru_peer_block'


def rms_norm(x, g):
    xf = x.astype(jnp.float32)
    y = xf * lax.rsqrt(jnp.mean(xf * xf, axis=-1, keepdims=True) + EPS)
    return (y * g.astype(jnp.float32)).astype(x.dtype)


def diff_attention(q, k, v, q_norm_g, k_norm_g, lambda_q1, lambda_k1, lambda_q2, lambda_k2,
                   subln_g, lam_init):
    b, s = q.shape[0], q.shape[1]
    n_blk = s // Q_BLOCK
    q = rms_norm(q, q_norm_g)
    k = rms_norm(k, k_norm_g)
    lam = (jnp.exp(jnp.sum(lambda_q1.astype(jnp.float32) * lambda_k1.astype(jnp.float32)))
           - jnp.exp(jnp.sum(lambda_q2.astype(jnp.float32) * lambda_k2.astype(jnp.float32)))
           + lam_init)
    scale = HEAD_DIM ** -0.5
    key_chunk = jnp.arange(s) // CHUNK
    q_blocks = q.reshape(b, n_blk, Q_BLOCK, N_ATTN_HEADS, 2, HEAD_DIM).transpose(1, 0, 2, 3, 4, 5)

    def one_block(args):
        q_blk, blk = args
        scores = jnp.einsum('bqhcd,bkhcd->bhcqk', q_blk, k).astype(jnp.float32) * scale
        q_chunk = (blk * Q_BLOCK + jnp.arange(Q_BLOCK)) // CHUNK
        visible = key_chunk[None, :] <= q_chunk[:, None]
        scores = jnp.where(visible, scores, NEG_INF)
        probs = jax.nn.softmax(scores, axis=-1)
        diff = probs[:, :, 0] - lam * probs[:, :, 1]
        return jnp.einsum('bhqk,bkhd->bqhd', diff.astype(v.dtype), v)

    o = lax.map(one_block, (q_blocks, jnp.arange(n_blk)))
    o = o.transpose(1, 0, 2, 3, 4).reshape(b, s, N_ATTN_HEADS, V_HEAD_DIM)
    o = rms_norm(o, subln_g) * (1.0 - lam_init)
    return o.reshape(b, s, ATTN_WIDTH)


def causal_depthwise_conv(x, w, bias):
    s = x.shape[1]
    xp = jnp.pad(x, ((0, 0), (CONV_WIDTH - 1, 0), (0, 0)))
    y = bias
    for tap in range(CONV_WIDTH):
        y = y + xp[:, tap:tap + s] * w[tap]
    return y


def rg_lru(x, w_a, b_a, w_x, b_x, rg_lambda):
    b, s, _ = x.shape
    xb = x.reshape(b, s, N_RNN_BLOCKS, RNN_BLOCK)
    gate_r = jax.nn.sigmoid(jnp.einsum('bsgi,gij->bsgj', xb, w_a).reshape(b, s, D_RNN) + b_a)
    gate_i = jax.nn.sigmoid(jnp.einsum('bsgi,gij->bsgj', xb, w_x).reshape(b, s, D_RNN) + b_x)
    log_a = -RG_C * gate_r.astype(jnp.float32) * jax.nn.softplus(-rg_lambda.astype(jnp.float32))
    a = jnp.exp(log_a)
    mult = jnp.sqrt(-jnp.expm1(2.0 * log_a))
    u = mult * (gate_i * x).astype(jnp.float32)

    def combine(left, right):
        a_l, u_l = left
        a_r, u_r = right
        return a_l * a_r, a_r * u_l + u_r

    _, h = lax.associative_scan(combine, (a, u), axis=1)
    return h.astype(x.dtype)


def peer_ffn(h, w_q, sub_keys, u_tab, v_tab):
    b, s, d = h.shape
    tokens = h.reshape((b * s) // PEER_BLOCK, PEER_BLOCK, d)

    def one_block(hb):
        q = (hb @ w_q).reshape(PEER_BLOCK, PEER_HEADS, 2, PEER_HALF)
        sc = jnp.einsum('thcd,hcnd->thcn', q, sub_keys).astype(jnp.float32)
        s1, i1 = lax.top_k(sc[:, :, 0], PEER_TOPK)
        s2, i2 = lax.top_k(sc[:, :, 1], PEER_TOPK)
        cand_s = (s1[..., :, None] + s2[..., None, :]).reshape(PEER_BLOCK, PEER_HEADS, PEER_TOPK * PEER_TOPK)
        cand_i = (i1[..., :, None] * PEER_N_KEYS + i2[..., None, :]).reshape(PEER_BLOCK, PEER_HEADS, PEER_TOPK * PEER_TOPK)
        top_s, pos = lax.top_k(cand_s, PEER_TOPK)
        idx = jnp.take_along_axis(cand_i, pos, axis=-1)
        g = jax.nn.softmax(top_s, axis=-1)
        u = u_tab[idx]
        act = jax.nn.gelu(jnp.einsum('thkd,td->thk', u, hb))
        wts = (g * act.astype(jnp.float32)).astype(hb.dtype)
        v = v_tab[idx]
        return jnp.einsum('thk,thkd->td', wts, v)

    out = lax.map(one_block, tokens)
    return out.reshape(b, s, d)


def hybrid_block(x, lam_init, norm1_g, w_in, b_gate, q_norm_g, k_norm_g, lambda_q1, lambda_k1,
                 lambda_q2, lambda_k2, subln_g, conv_w, conv_b, w_rg_a, b_rg_a, w_rg_x, b_rg_x,
                 rg_lambda, w_br_attn, w_br_rnn, w_out, norm2_g, w_peer_q, peer_sub_keys,
                 peer_u, peer_v):
    b, s, _ = x.shape
    h = rms_norm(x, norm1_g)
    z = h @ w_in
    q, k, v, xr, yr, gate_logits = jnp.split(z, IN_SPLITS, axis=-1)
    q = q.reshape(b, s, N_ATTN_HEADS, 2, HEAD_DIM)
    k = k.reshape(b, s, N_ATTN_HEADS, 2, HEAD_DIM)
    v = v.reshape(b, s, N_ATTN_HEADS, V_HEAD_DIM)
    attn = diff_attention(q, k, v, q_norm_g, k_norm_g, lambda_q1, lambda_k1, lambda_q2,
                          lambda_k2, subln_g, lam_init)
    rnn = rg_lru(causal_depthwise_conv(xr, conv_w, conv_b), w_rg_a, b_rg_a, w_rg_x, b_rg_x,
                 rg_lambda) * jax.nn.gelu(yr)
    gates = jax.nn.sigmoid(gate_logits + b_gate).reshape(b, s, N_BRANCH, D_MODEL)
    merged = gates[:, :, 0] * (attn @ w_br_attn) + gates[:, :, 1] * (rnn @ w_br_rnn)
    x = x + merged @ w_out
    x = x + peer_ffn(rms_norm(x, norm2_g), w_peer_q, peer_sub_keys, peer_u, peer_v)
    return x


def setup_inputs(seed: int = 0) -> dict:
    key = jax.random.key(seed)
    ks = jax.random.split(key, 26)
    L = DEPTH

    def nrm(k, shape, scale):
        return jax.random.normal(k, shape, jnp.float32) * scale

    a0 = jax.random.uniform(ks[17], (L, D_RNN), jnp.float32, 0.9, 0.999)
    return {
        'x': nrm(ks[0], (BATCH, SEQ, D_MODEL), 1.0),
        'norm1_g': 1.0 + nrm(ks[1], (L, D_MODEL), 0.02),
        'w_in': nrm(ks[2], (L, D_MODEL, IN_COLS), D_MODEL ** -0.5),
        'b_gate': nrm(ks[3], (L, N_BRANCH * D_MODEL), 0.02),
        'q_norm_g': 1.0 + nrm(ks[4], (L, HEAD_DIM), 0.02),
        'k_norm_g': 1.0 + nrm(ks[5], (L, HEAD_DIM), 0.02),
        'lambda_q1': nrm(ks[6], (L, HEAD_DIM), 0.1),
        'lambda_k1': nrm(ks[7], (L, HEAD_DIM), 0.1),
        'lambda_q2': nrm(ks[8], (L, HEAD_DIM), 0.1),
        'lambda_k2': nrm(ks[9], (L, HEAD_DIM), 0.1),
        'subln_g': 1.0 + nrm(ks[10], (L, V_HEAD_DIM), 0.02),
        'conv_w': nrm(ks[11], (L, CONV_WIDTH, D_RNN), CONV_WIDTH ** -0.5),
        'conv_b': nrm(ks[12], (L, D_RNN), 0.02),
        'w_rg_a': nrm(ks[13], (L, N_RNN_BLOCKS, RNN_BLOCK, RNN_BLOCK), RNN_BLOCK ** -0.5),
        'b_rg_a': nrm(ks[14], (L, D_RNN), 0.02),
        'w_rg_x': nrm(ks[15], (L, N_RNN_BLOCKS, RNN_BLOCK, RNN_BLOCK), RNN_BLOCK ** -0.5),
        'b_rg_x': nrm(ks[16], (L, D_RNN), 0.02),
        'rg_lambda': jnp.log(a0) - jnp.log1p(-a0),
        'w_br_attn': nrm(ks[18], (L, ATTN_WIDTH, D_MODEL), ATTN_WIDTH ** -0.5),
        'w_br_rnn': nrm(ks[19], (L, D_RNN, D_MODEL), D_RNN ** -0.5),
        'w_out': nrm(ks[20], (L, D_MODEL, D_MODEL), D_MODEL ** -0.5),
        'norm2_g': 1.0 + nrm(ks[21], (L, D_MODEL), 0.02),
        'w_peer_q': nrm(ks[22], (L, D_MODEL, PEER_HEADS * PEER_QDIM), D_MODEL ** -0.5),
        'peer_sub_keys': nrm(ks[23], (L, PEER_HEADS, 2, PEER_N_KEYS, PEER_HALF), PEER_HALF ** -0.5),
        'peer_u': nrm(ks[24], (L, PEER_N_EXPERTS, D_MODEL), D_MODEL ** -0.5),
        'peer_v': nrm(ks[25], (L, PEER_N_EXPERTS, D_MODEL), PEER_HEADS ** -0.5),
    }


def reference(x, norm1_g, w_in, b_gate, q_norm_g, k_norm_g, lambda_q1, lambda_k1, lambda_q2,
              lambda_k2, subln_g, conv_w, conv_b, w_rg_a, b_rg_a, w_rg_x, b_rg_x, rg_lambda,
              w_br_attn, w_br_rnn, w_out, norm2_g, w_peer_q, peer_sub_keys, peer_u, peer_v):
    for layer in range(DEPTH):
        lam_init = 0.8 - 0.6 * math.exp(-0.3 * layer)
        x = hybrid_block(x, lam_init, norm1_g[layer], w_in[layer], b_gate[layer], q_norm_g[layer],
                         k_norm_g[layer], lambda_q1[layer], lambda_k1[layer], lambda_q2[layer],
                         lambda_k2[layer], subln_g[layer], conv_w[layer], conv_b[layer],
                         w_rg_a[layer], b_rg_a[layer], w_rg_x[layer], b_rg_x[layer],
                         rg_lambda[layer], w_br_attn[layer], w_br_rnn[layer], w_out[layer],
                         norm2_g[layer], w_peer_q[layer], peer_sub_keys[layer], peer_u[layer],
                         peer_v[layer])
    return x
```

```python
import contextlib
import numpy as np
import concourse.bass as bass
import concourse.mybir as mybir
from concourse.bass_utils import run_bass_kernel_spmd

F32 = mybir.dt.float32
BF16 = mybir.dt.bfloat16
AF = mybir.ActivationFunctionType
ALU = mybir.AluOpType

D = 1024
KC = 8
EPS = 1e-6
LAM_INIT = 0.2
N_ET = 128
EG = 4


class Tok:
    __slots__ = ("key", "sem", "val")

    def __init__(self, key, sem, val):
        self.key, self.sem, self.val = key, sem, val


class Buf:
    def __init__(self, name):
        self.name = name
        self.w = None
        self.r = {}
        self.dsem = {}
        self.dcnt = {}


class Eng:
    def __init__(self, K, eng, name, is_pe=False, counted=True):
        self.K, self.eng, self.name, self.is_pe, self.counted = K, eng, name, is_pe, counted
        self.epoch = 0
        self.count = 0
        self.seen = {}
        self.sem = K.new_sem(name + "0") if counted else None

    @property
    def key(self):
        return "%s@%d" % (self.name, self.epoch)

    def bump(self):
        if self.count >= 20000:
            self.epoch += 1
            self.count = 0
            self.sem = self.K.new_sem("%s%d" % (self.name, self.epoch))


class Kern:
    def __init__(self, nc, es):
        self.nc, self.es = nc, es
        self.nsem = 0
        self.pe = Eng(self, nc.tensor, "pe", is_pe=True)
        self.act = Eng(self, nc.scalar, "act")
        self.dve = Eng(self, nc.vector, "dve")
        self.pool = Eng(self, nc.gpsimd, "pool")
        self.sp = Eng(self, nc.sync, "sp", counted=False)
        self.engs = [self.pe, self.act, self.dve, self.pool, self.sp]
        self.dma_toks = {}

    def new_sem(self, name):
        self.nsem += 1
        return self.es.enter_context(self.nc.semaphore("s_%s_%d" % (name, self.nsem)))

    def _deps(self, E, R, W):
        deps = {}

        def add(t):
            if t is None:
                return
            o = deps.get(t.key)
            if o is None or o.val < t.val:
                deps[t.key] = t
        for b in R:
            add(b.w)
        for b in W:
            add(b.w)
            for t in b.r.values():
                add(t)
        for key, t in deps.items():
            if E.is_pe and key.startswith("pe@"):
                continue
            if E.seen.get(key, 0) < t.val:
                E.eng.wait_ge(t.sem, t.val)
                E.seen[key] = t.val

    def op(self, E, fn, R=(), W=()):
        self._deps(E, R, W)
        ins = fn()
        E.bump()
        E.count += 1
        ins.then_inc(E.sem, 1)
        tok = Tok(E.key, E.sem, E.count)
        for b in R:
            b.r[tok.key] = tok
        for b in W:
            b.w = tok
            b.r = {}
        return ins

    def dma(self, Q, out, in_, R=(), W=(), own=None):
        self._deps(Q, R, W)
        b = own
        qn = Q.name
        if qn not in b.dsem:
            b.dsem[qn] = self.new_sem("d")
            b.dcnt[qn] = 0
        b.dcnt[qn] += 1
        Q.eng.dma_start(out=out, in_=in_).then_inc(b.dsem[qn], 16)
        tok = Tok("d:" + b.name + ":" + qn, b.dsem[qn], 16 * b.dcnt[qn])
        self.dma_toks[tok.key] = tok
        for x in R:
            x.r[tok.key] = tok
        for x in W:
            x.w = tok
            x.r = {}

    def barrier(self):
        toks = []
        for P in (self.pe, self.act, self.dve, self.pool):
            if P.count > 0:
                toks.append(Tok(P.key, P.sem, P.count))
        toks += list(self.dma_toks.values())
        for E in self.engs:
            for t in toks:
                if E.seen.get(t.key, 0) < t.val:
                    E.eng.wait_ge(t.sem, t.val)
                    E.seen[t.key] = t.val


def _view(reg, boff, dt, shape):
    esz = 2 if dt == BF16 else 4
    n = 1
    for s in shape[1:]:
        n *= s
    nb = n * esz
    assert boff % 4 == 0 and nb % 4 == 0
    assert boff + nb <= reg.shape[1] * 4, ("region overflow", boff, nb, reg.shape)
    v = reg[:, boff // 4:(boff + nb) // 4]
    if dt != F32:
        v = v.bitcast(dt)
    fs = shape[1:]
    if len(fs) == 2:
        v = v.rearrange("p (a b) -> p a b", a=fs[0])
    elif len(fs) == 3:
        v = v.rearrange("p (a b c) -> p a b c", a=fs[0], b=fs[1])
    elif len(fs) == 4:
        v = v.rearrange("p (a b c d) -> p a b c d", a=fs[0], b=fs[1], c=fs[2])
    return v


class Bump:
    def __init__(self, reg):
        self.reg = reg
        self.off = 0

    def reset(self):
        self.off = 0

    def take(self, dt, shape):
        esz = 2 if dt == BF16 else 4
        n = 1
        for s in shape[1:]:
            n *= s
        nb = (n * esz + 31) // 32 * 32
        v = _view(self.reg, self.off, dt, shape) if (n * esz) % 4 == 0 else None
        self.off += nb
        return v


def build_nc(NT, debug=False):
    NO = NT // 2
    S = NT * 128
    SO = NO * 128
    NB = S // 512
    NOB = SO // 512
    assert S % 512 == 0 and SO % 512 == 0

    nc = bass.Bass("TRN2", target_bir_lowering=False)

    def din(name, shape, dt=F32):
        return nc.dram_tensor(name, list(shape), dt, kind="ExternalInput").ap()

    x_all = din("x_all", [S, D])
    x_own = din("x_own", [SO, D])
    w_in_blk = din("w_in_blk", [60, 128, KC, 128])
    g1 = din("g1", [D])
    g2 = din("g2", [D])
    cols = din("cols", [128, 32])
    lamv = din("lamv", [256])
    sublng = din("sublng", [128])
    rnnvec = din("rnnvec", [128, 10, 8])
    wbd = din("wbd", [128, 10, 2, 128])
    wpa = din("wpa", [8, 128, 8, 128])
    wpr = din("wpr", [8, 128, 10, 128])
    wout = din("wout", [128, KC, D])
    wq = din("wq", [16, 128, KC, 128])
    keysT = din("keysT", [128, 16, 128])
    u_t = din("u_t", [N_ET, 128, KC, 128])
    v_tab = din("v_tab", [N_ET * 128, D])
    mask2 = din("mask2", [128, 256])
    ident_d = din("ident", [128, 128])
    bones_d = din("bones", [128, 128])
    iota_d = din("iota", [128, 128])
    hm_d = din("hm", [128, 8])
    out_own = nc.dram_tensor("out_own", [SO, D], F32, kind="ExternalOutput").ap()
    u_bf = nc.dram_tensor("u_bf16", [N_ET, 128, KC, 128], BF16, kind="Internal").ap()
    v_bf = nc.dram_tensor("v_bf16", [N_ET * 128, D], BF16, kind="Internal").ap()
    dbg = None
    if debug:
        dbg = nc.dram_tensor("dbg_x2", [SO, D], F32, kind="ExternalOutput").ap()

    with contextlib.ExitStack() as es:
        K = Kern(nc, es)
        PE, ACT, DVE, POOL, SP = K.pe, K.act, K.dve, K.pool, K.sp

        def sb(name, n_f32):
            return es.enter_context(nc.sbuf_tensor(name, [128, n_f32], F32))[:, :]

        R0 = sb("R0", 32768)
        RA = R0[:, 0:16384]
        RB = R0[:, 16384:24576]
        RC = R0[:, 24576:32768]
        RBC = R0[:, 16384:32768]
        RD = sb("RD", 10240)
        RE = sb("RE", 8192)
        CT = sb("CT", 1280)
        banks = [es.enter_context(nc.psum_tensor("ps%d" % i, [128, 512], F32))[:, :] for i in range(8)]
        PB = [Buf("ps%d" % i) for i in range(8)]

        def mm(out, lhsT, rhs, start, stop, R, W):
            K.op(PE, lambda: nc.tensor.matmul(out, lhsT=lhsT, rhs=rhs, start=start, stop=stop), R, W)

        def tr(out, in_, R, W):
            K.op(PE, lambda: nc.tensor.transpose(out, in_, ident), R + [B_const], W)

        def act(out, in_, func, R, W, bias=None, scale=None, accum=None):
            kw = {}
            if bias is not None:
                kw["bias"] = bias
            if scale is not None:
                kw["scale"] = scale
            if accum is not None:
                kw["accum_out"] = accum
            K.op(ACT, lambda: nc.scalar.activation(out=out, in_=in_, func=func, **kw), R, W)

        def ts(E, out, in0, s1, s2, op0, op1, R, W):
            if op1 is None:
                K.op(E, lambda: E.eng.tensor_scalar(out=out, in0=in0, scalar1=s1, scalar2=None, op0=op0), R, W)
            else:
                K.op(E, lambda: E.eng.tensor_scalar(out=out, in0=in0, scalar1=s1, scalar2=s2, op0=op0, op1=op1), R, W)

        def stt(out, in0, scalar, in1, op0, op1, R, W):
            K.op(DVE, lambda: nc.vector.scalar_tensor_tensor(out=out, in0=in0, scalar=scalar, in1=in1, op0=op0, op1=op1), R, W)

        def tt(E, out, in0, in1, op, R, W):
            K.op(E, lambda: E.eng.tensor_tensor(out=out, in0=in0, in1=in1, op=op), R, W)

        def cp(E, out, in_, R, W):
            if E is ACT:
                K.op(E, lambda: nc.scalar.copy(out=out, in_=in_), R, W)
            else:
                K.op(E, lambda: E.eng.tensor_copy(out=out, in_=in_), R, W)

        def recip(out, in_, R, W):
            K.op(DVE, lambda: nc.vector.reciprocal(out=out, in_=in_), R, W)

        def memset(E, ap, val, W):
            K.op(E, lambda: E.eng.memset(ap, val), [], W)

        cb = Bump(CT)
        ident = cb.take(F32, [128, 128])
        identb = cb.take(BF16, [128, 128])
        bonesb = cb.take(BF16, [128, 128])
        maskb = cb.take(BF16, [128, 256])
        colt = cb.take(F32, [128, 32])
        rnv = cb.take(F32, [128, 10, 8])
        sgrep = cb.take(F32, [128, 128])
        lamt = cb.take(F32, [128, 256])
        misc = cb.take(F32, [128, 64])
        iotat = cb.take(F32, [128, 128])
        rn2 = cb.take(F32, [128, 40])
        B_rn2 = Buf("rn2")
        hmb = cb.take(BF16, [128, 8])
        B_const = Buf("const")
        K.dma(SP, ident, ident_d[:, :], W=[B_const], own=B_const)
        K.dma(POOL, identb, ident_d[:, :], W=[B_const], own=B_const)
        K.dma(POOL, bonesb, bones_d[:, :], W=[B_const], own=B_const)
        K.dma(POOL, maskb, mask2[:, :], W=[B_const], own=B_const)
        K.dma(SP, colt, cols[:, :], W=[B_const], own=B_const)
        K.dma(SP, iotat, iota_d[:, :], W=[B_const], own=B_const)
        K.dma(POOL, hmb, hm_d[:, :], W=[B_const], own=B_const)
        K.dma(SP, rnv, rnnvec[:, :, :], W=[B_const], own=B_const)
        K.dma(SP, sgrep, sublng.partition_broadcast(128), W=[B_const], own=B_const)
        K.dma(SP, lamt, lamv.partition_broadcast(128), W=[B_const], own=B_const)
        B_misc = Buf("misc")
        epsc = misc[:, 0:1]
        gq8 = misc[:, 1:2]
        nlam = misc[:, 2:3]
        sa = misc[:, 8:18]
        sa2 = misc[:, 18:28]
        memset(DVE, misc[:, 0:1], EPS, [B_misc])
        mhalf = misc[:, 30:31]
        memset(DVE, misc[:, 30:31], -0.5, [B_misc])
        ts(DVE, gq8, colt[:, 16:17], 0.125, None, ALU.mult, None, [B_const, B_misc], [B_misc])
        junk64 = misc[:, 32:96] if False else None
        lw = lamt
        tt(DVE, lamt[:, 0:64], lamt[:, 0:64], lamt[:, 64:128], ALU.mult, [B_const], [B_const])
        tt(DVE, lamt[:, 128:192], lamt[:, 128:192], lamt[:, 192:256], ALU.mult, [B_const], [B_const])
        K.op(DVE, lambda: nc.vector.reduce_sum(out=misc[:, 3:4], in_=lamt[:, 0:64], axis=mybir.AxisListType.X), [B_const, B_misc], [B_misc])
        K.op(DVE, lambda: nc.vector.reduce_sum(out=misc[:, 4:5], in_=lamt[:, 128:192], axis=mybir.AxisListType.X), [B_const, B_misc], [B_misc])
        act(misc[:, 3:5], misc[:, 3:5], AF.Exp, [B_misc], [B_misc])
        tt(DVE, misc[:, 5:6], misc[:, 4:5], misc[:, 3:4], ALU.subtract, [B_misc], [B_misc])
        ts(DVE, nlam, misc[:, 5:6], -LAM_INIT, None, ALU.add, None, [B_misc], [B_misc])
        act(sa, rnv[:, :, 7], AF.Exp, [B_const, B_misc], [B_misc], scale=-1.0)
        ts(DVE, sa, sa, 1.0, None, ALU.add, None, [B_misc], [B_misc])
        act(sa, sa, AF.Ln, [B_misc], [B_misc])
        ts(DVE, sa2, sa, -16.0, None, ALU.mult, None, [B_misc], [B_misc])
        ts(DVE, sa, sa, -8.0, None, ALU.mult, None, [B_misc], [B_misc])
        ts(DVE, sgrep, sgrep, 1.0 - LAM_INIT, None, ALU.mult, None, [B_const], [B_const])
        CR = [B_const, B_misc]

        def load_wblk(dst, src_ap, buf):
            K.dma(POOL, dst, src_ap, W=[buf], own=buf)

        def norm_T(xt, Bx, grep, Bg, scratch, Bs, dst_fn, Bdst, pb0, pb1):
            act(scratch["junk"], xt, AF.Square, [Bx], [Bs], accum=scratch["ss"])
            ts(DVE, scratch["ms"], scratch["ss"], 1.0 / D, EPS, ALU.mult, ALU.add, [Bs], [Bs])
            act(scratch["ms"], scratch["ms"], AF.Sqrt, [Bs], [Bs])
            recip(scratch["rs"], scratch["ms"], [Bs], [Bs])
            stt(scratch["xn"], xt, scratch["rs"], grep, ALU.mult, ALU.mult, [Bx, Bg, Bs], [Bs])
            for half, pb in ((0, pb0), (1, pb1)):
                for q in range(4):
                    kc = half * 4 + q
                    tr(banks[pb][:, q * 128:(q + 1) * 128], scratch["xn"][:, kc * 128:(kc + 1) * 128], [Bs], [PB[pb]])
                E = ACT if half == 0 else DVE
                cp(E, dst_fn(half), banks[pb].rearrange("p (a b) -> p a b", a=4), [PB[pb]], [Bdst])

        def select_own(dst, src_pairs, Bsrc, Bdst):
            ts(DVE, dst, src_pairs[:, :, 0, :], colt[:, 18:19], None, ALU.mult, None, [Bsrc, B_const], [Bdst])
            stt(dst, src_pairs[:, :, 1, :], colt[:, 19:20], dst, ALU.mult, ALU.add, [Bsrc, B_const, Bdst], [Bdst])

        hT_all = _view(RA, 0, BF16, [128, KC, S])
        B_hT = Buf("hT_all")
        eb = Bump(RE)
        g1rep = eb.take(F32, [128, D])
        B_g1 = Buf("g1rep")
        K.dma(SP, g1rep, g1.partition_broadcast(128), W=[B_g1], own=B_g1)
        xs = [eb.take(F32, [128, D]) for _ in range(2)]
        Bxs = [Buf("xs0"), Buf("xs1")]
        scr = []
        Bscr = []
        for i in range(2):
            scr.append({"xn": eb.take(F32, [128, D]), "junk": eb.take(BF16, [128, D]), "st": eb.take(F32, [128, 8])})
            scr[i]["ss"] = scr[i]["st"][:, 0:1]
            scr[i]["ms"] = scr[i]["st"][:, 1:2]
            scr[i]["rs"] = scr[i]["st"][:, 2:3]
            Bscr.append(Buf("scr%d" % i))
        def p1_a(t):
            s_ = t % 2
            sc_, Bs_ = scr[s_], Bscr[s_]
            K.dma(SP, xs[s_], x_all[t * 128:(t + 1) * 128, :], W=[Bxs[s_]], own=Bxs[s_])
            act(sc_["junk"], xs[s_], AF.Square, [Bxs[s_]], [Bs_], accum=sc_["ss"])
            ts(DVE, sc_["ms"], sc_["ss"], 1.0 / D, EPS, ALU.mult, ALU.add, [Bs_], [Bs_])
            act(sc_["ms"], sc_["ms"], AF.Sqrt, [Bs_], [Bs_])
            recip(sc_["rs"], sc_["ms"], [Bs_], [Bs_])
            stt(sc_["xn"], xs[s_], sc_["rs"], g1rep, ALU.mult, ALU.mult, [Bxs[s_], B_g1, Bs_], [Bs_])

        def p1_b(t):
            s_ = t % 2
            sc_, Bs_ = scr[s_], Bscr[s_]
            for half in range(2):
                pb = 2 * s_ + half
                for q in range(4):
                    kc = half * 4 + q
                    tr(banks[pb][:, q * 128:(q + 1) * 128], sc_["xn"][:, kc * 128:(kc + 1) * 128], [Bs_], [PB[pb]])
                cp(ACT if half == 0 else DVE, hT_all[:, half * 4:half * 4 + 4, t * 128:(t + 1) * 128],
                   banks[pb].rearrange("p (a b) -> p a b", a=4), [PB[pb]], [B_hT])

        for t in range(NT + 1):
            if t < NT:
                p1_a(t)
            if t >= 1:
                p1_b(t - 1)
        K.barrier()

        hT_own = _view(RB, 0, BF16, [128, KC, SO])
        B_hTo = Buf("hT_own")
        rnnT = _view(RD, 0, BF16, [128, 10, SO])
        B_rnnT = Buf("rnnT")
        GR = _view(RC, 0, BF16, [128, 8, SO])
        B_GR = Buf("GR")
        for kc in range(KC):
            select_own(hT_own[:, kc, :].rearrange("p (n c) -> p n c", c=128),
                       hT_all[:, kc, :].rearrange("p (n two c) -> p n two c", two=2, c=128), B_hT, B_hTo)
        eb = Bump(RE)
        wxr = [eb.take(BF16, [128, KC, 128]) for _ in range(2)]
        wyr = [eb.take(BF16, [128, KC, 128]) for _ in range(2)]
        Bwxr = [Buf("wxr0"), Buf("wxr1")]
        Bwyr = [Buf("wyr0"), Buf("wyr1")]
        wbdt = eb.take(BF16, [128, 10, 2, 128])
        B_wbd = Buf("wbd")
        K.dma(POOL, wbdt, wbd[:, :, :, :], W=[B_wbd], own=B_wbd)
        NRB = 2
        rb = []
        eb2 = Bump(RC)
        for i in range(NRB):
            if i == 1:
                eb = eb2
            d = {"xr": eb.take(F32, [128, 516]), "xc": eb.take(F32, [128, 512]), "xcb": eb.take(BF16, [128, 512]),
                 "gr": eb.take(F32, [128, 512]), "gi": eb.take(F32, [128, 512]), "a": eb.take(F32, [128, 512]),
                 "m": eb.take(F32, [128, 512]), "h": eb.take(F32, [128, 512]),
                 "gy": eb.take(F32, [128, 256]), "hs": eb.take(F32, [128, 256])}
            d["B"] = {k: Buf("rb%d_%s" % (i, k)) for k in ("xr", "xc", "xcb", "gr", "gi", "a", "m", "h", "gy", "hs")}
            rb.append(d)
        zcol = misc[:, 29:30]
        memset(DVE, misc[:, 29:30], 0.0, [B_misc])
        halfc = misc[:, 31:32]
        memset(DVE, misc[:, 31:32], 0.5, [B_misc])
        q25c = misc[:, 6:7]
        memset(DVE, misc[:, 6:7], 0.25, [B_misc])
        ts(DVE, rn2[:, 0:10], rnv[:, :, 5], 0.5, None, ALU.mult, None, [B_const], [B_rn2])
        ts(DVE, rn2[:, 10:20], rnv[:, :, 6], 0.5, None, ALU.mult, None, [B_const], [B_rn2])
        ts(DVE, rn2[:, 20:30], sa, 0.5, None, ALU.mult, None, [B_misc], [B_rn2])
        ts(DVE, rn2[:, 30:40], sa2, 0.5, None, ALU.mult, None, [B_misc], [B_rn2])
        GK, GC = 0.7978845608028654, 0.044715
        its = [(j, tb) for j in range(10) for tb in range(NB)]

        def stage_a(n):
            j, tb = its[n]
            s = j % 2
            if tb == 0:
                load_wblk(wxr[s], w_in_blk[24 + j], Bwxr[s])
                load_wblk(wyr[s], w_in_blk[34 + j], Bwyr[s])
            r, rp = rb[n % NRB], rb[(n - 1) % NRB]
            B = r["B"]
            pbx = n % 2
            for kc in range(KC):
                mm(banks[pbx], wxr[s][:, kc, :], hT_all[:, kc, tb * 512:(tb + 1) * 512], kc == 0, kc == KC - 1,
                   [Bwxr[s], B_hT], [PB[pbx]])
            if tb == 0:
                memset(DVE, r["xr"][:, 0:3], 0.0, [B["xr"]])
            else:
                cp(DVE, r["xr"][:, 0:3], rp["xr"][:, 512:515], [rp["B"]["xr"]], [B["xr"]])
            cp(ACT, r["xr"][:, 3:515], banks[pbx], [PB[pbx]], [B["xr"]])
            ts(POOL, r["xc"], r["xr"][:, 0:512], rnv[:, j, 0:1], rnv[:, j, 4:5], ALU.mult, ALU.add, [B["xr"], B_const], [B["xc"]])
            for tap in (1, 2, 3):
                stt(r["xc"], r["xr"][:, tap:tap + 512], rnv[:, j, tap:tap + 1], r["xc"], ALU.mult, ALU.add,
                    [B["xr"], B_const, B["xc"]], [B["xc"]])
            cp(POOL, r["xcb"], r["xc"], [B["xc"]], [B["xcb"]])
            oc0 = tb * 256
            for kc in range(KC):
                mm(banks[6 + pbx][:, 0:256], wyr[s][:, kc, :], hT_own[:, kc, oc0:oc0 + 256], kc == 0, kc == KC - 1,
                   [Bwyr[s], B_hTo], [PB[6 + pbx]])

        def stage_a2(n):
            j, tb = its[n]
            r = rb[n % NRB]
            B = r["B"]
            pbx = n % 2
            mm(banks[2 + pbx], wbdt[:, j, 0, :], r["xcb"], True, True, [B_wbd, B["xcb"]], [PB[2 + pbx]])
            mm(banks[4 + pbx], wbdt[:, j, 1, :], r["xcb"], True, True, [B_wbd, B["xcb"]], [PB[4 + pbx]])

        def stage_b(n):
            j, tb = its[n]
            r, rp = rb[n % NRB], rb[(n - 1) % NRB]
            B = r["B"]
            pbx = n % 2
            RN = [B_rn2]
            act(r["gr"], banks[2 + pbx], AF.Tanh, [PB[2 + pbx]] + RN, [B["gr"]], bias=rn2[:, j:j + 1], scale=0.5)
            act(r["gi"], banks[4 + pbx], AF.Tanh, [PB[4 + pbx]] + RN, [B["gi"]], bias=rn2[:, 10 + j:11 + j], scale=0.5)
            act(r["a"], r["gr"], AF.Exp, [B["gr"]] + RN, [B["a"]], scale=rn2[:, 20 + j:21 + j], bias=rn2[:, 20 + j:21 + j])
            act(r["m"], r["gr"], AF.Exp, [B["gr"]] + RN, [B["m"]], scale=rn2[:, 30 + j:31 + j], bias=rn2[:, 30 + j:31 + j])
            act(r["m"], r["m"], AF.Sqrt, [B["m"], B_misc], [B["m"]], scale=-0.25, bias=q25c)
            stt(r["gi"], r["gi"], 1.0, r["xc"], ALU.add, ALU.mult, [B["gi"], B["xc"]], [B["gi"]])
            tt(DVE, r["gi"], r["gi"], r["m"], ALU.mult, [B["gi"], B["m"]], [B["gi"]])
            init = zcol if tb == 0 else rp["h"][:, 511:512]
            RI = [B_misc] if tb == 0 else [rp["B"]["h"]]
            K.op(DVE, lambda: nc.vector.tensor_tensor_scan(out=r["h"], data0=r["a"], data1=r["gi"], initial=init,
                                                           op0=ALU.mult, op1=ALU.add),
                 [B["a"], B["gi"]] + RI, [B["h"]])
            yps = banks[6 + pbx][:, 0:256]
            act(r["gy"], yps, AF.Square, [PB[6 + pbx]], [B["gy"]])
            ts(DVE, r["gy"], r["gy"], GC, 1.0, ALU.mult, ALU.add, [B["gy"]], [B["gy"]])
            tt(DVE, r["gy"], r["gy"], yps, ALU.mult, [B["gy"], PB[6 + pbx]], [B["gy"]])
            act(r["gy"], r["gy"], AF.Tanh, [B["gy"]], [B["gy"]], scale=GK)
            stt(r["gy"], r["gy"], 1.0, yps, ALU.add, ALU.mult, [B["gy"], PB[6 + pbx]], [B["gy"]])
            select_own(r["hs"].rearrange("p (n c) -> p n c", c=128),
                       r["h"].rearrange("p (n two c) -> p n two c", two=2, c=128), B["h"], B["hs"])
            oc0 = tb * 256
            stt(rnnT[:, j, oc0:oc0 + 256], r["hs"], 0.5, r["gy"], ALU.mult, ALU.mult, [B["hs"], B["gy"]], [B_rnnT])

        stage_a(0)
        for n in range(len(its)):
            if n + 1 < len(its):
                stage_a(n + 1)
            stage_a2(n)
            stage_b(n)
        K.barrier()
        eb = Bump(RE)
        wprs = [eb.take(BF16, [128, 10, 128]) for _ in range(2)]
        wgs = [eb.take(BF16, [128, KC, 128]) for _ in range(2)]
        Bwprs = [Buf("wpr0"), Buf("wpr1")]
        Bwgs = [Buf("wg0"), Buf("wg1")]
        gsg = [eb.take(F32, [128, 512]) for _ in range(2)]
        Bgsg = [Buf("gsg0"), Buf("gsg1")]
        def p2_a(n):
            oc, ob = divmod(n, NOB)
            s_ = oc % 2
            q = n % 2
            if ob == 0:
                K.dma(POOL, wprs[s_], wpr[oc], W=[Bwprs[s_]], own=Bwprs[s_])
                load_wblk(wgs[s_], w_in_blk[52 + oc], Bwgs[s_])
            cs = slice(ob * 512, (ob + 1) * 512)
            for j in range(10):
                mm(banks[q], wprs[s_][:, j, :], rnnT[:, j, cs], j == 0, j == 9, [Bwprs[s_], B_rnnT], [PB[q]])
            for kc in range(KC):
                mm(banks[2 + q], wgs[s_][:, kc, :], hT_own[:, kc, cs], kc == 0, kc == KC - 1, [Bwgs[s_], B_hTo], [PB[2 + q]])

        def p2_b(n):
            oc, ob = divmod(n, NOB)
            q = n % 2
            cs = slice(ob * 512, (ob + 1) * 512)
            act(gsg[q], banks[2 + q], AF.Sigmoid, [PB[2 + q], B_const], [Bgsg[q]], bias=colt[:, 8 + oc:9 + oc])
            tt(DVE, GR[:, oc, cs], banks[q], gsg[q], ALU.mult, [PB[q], Bgsg[q]], [B_GR])

        for n in range(8 * NOB + 1):
            if n < 8 * NOB:
                p2_a(n)
            if n >= 1:
                p2_b(n - 1)
        K.barrier()

        attnT = _view(RB, 0, BF16, [128, 8, SO])
        B_attnT = Buf("attnT")
        db = Bump(RD)
        KT = db.take(BF16, [128, S])
        Qall = db.take(BF16, [128, S])
        Qown = db.take(BF16, [128, SO])
        Vh = db.take(BF16, [128, NT, 130])
        B_KT, B_Qall, B_Qown, B_Vh = Buf("KT"), Buf("Qall"), Buf("Qown"), Buf("Vh")
        memset(DVE, Vh[:, :, 128:130], 1.0, [B_Vh])
        eb = Bump(RE)
        wq_s = [eb.take(BF16, [128, KC, 128]) for _ in range(2)]
        wk_s = [eb.take(BF16, [128, KC, 128]) for _ in range(2)]
        wv_s = [eb.take(BF16, [128, KC, 128]) for _ in range(2)]
        Bwq = [Buf("wq0"), Buf("wq1")]
        Bwk = [Buf("wk0"), Buf("wk1")]
        Bwv = [Buf("wv0"), Buf("wv1")]
        sqb = [eb.take(BF16, [128, 512]) for _ in range(2)]
        sdb = [eb.take(F32, [128, 512]) for _ in range(2)]
        Bsq = [Buf("sq0"), Buf("sq1")]
        Bsd = [Buf("sd0"), Buf("sd1")]
        NPT = 4
        pT = [eb.take(BF16, [128, 512]) for _ in range(NPT)]
        BpT = [Buf("pT%d" % i) for i in range(NPT)]
        o0 = [eb.take(F32, [128, 128]) for _ in range(2)]
        dif = [eb.take(F32, [128, 128]) for _ in range(2)]
        Bo0 = [Buf("o00"), Buf("o01")]
        Bdif = [Buf("dif0"), Buf("dif1")]
        ast = [eb.take(F32, [128, 8]) for _ in range(2)]
        Bast = [Buf("ast0"), Buf("ast1")]
        ajunk = eb.take(BF16, [128, 128])
        B_aj = Buf("ajunk")
        NSTG = 3
        stg = [eb.take(BF16, [128, D]) for _ in range(NSTG)]
        Bstg = [Buf("stg%d" % i) for i in range(NSTG)]
        conv_done = [Buf("conv_done%d" % i) for i in range(NSTG)]
        conv_state = [0]

        def convert_next():
            n_ = conv_state[0]
            if n_ >= 2 * N_ET:
                return
            conv_state[0] += 1
            sl = n_ % NSTG
            e_ = n_ // 2
            if n_ % 2 == 0:
                K.dma(POOL, stg[sl].rearrange("p (k e) -> p k e", k=KC), u_t[e_], W=[Bstg[sl]], own=Bstg[sl])
                K.dma(SP, u_bf[e_], stg[sl].rearrange("p (k e) -> p k e", k=KC), R=[Bstg[sl]], W=[conv_done[sl]], own=Bstg[sl])
            else:
                K.dma(POOL, stg[sl], v_tab[e_ * 128:(e_ + 1) * 128, :], W=[Bstg[sl]], own=Bstg[sl])
                K.dma(SP, v_bf[e_ * 128:(e_ + 1) * 128, :], stg[sl], R=[Bstg[sl]], W=[conv_done[sl]], own=Bstg[sl])

        def qk_proj(w, Bw, gcol, dst, Bdst, itc):
            pbs = []

            def proj(tb):
                pb = itc[0] % 3
                itc[0] += 1
                pbs.append(pb)
                cs = slice(tb * 512, (tb + 1) * 512)
                for kc in range(KC):
                    mm(banks[pb], w[:, kc, :], hT_all[:, kc, cs], kc == 0, kc == KC - 1, [Bw, B_hT], [PB[pb]])

            def fin(tb):
                pb = pbs[tb]
                q = tb % 2
                cs = slice(tb * 512, (tb + 1) * 512)
                act(sqb[q], banks[pb], AF.Square, [PB[pb]], [Bsq[q]])
                mm(banks[3], bonesb, sqb[q], True, True, [B_const, Bsq[q]], [PB[3]])
                act(sdb[q], banks[3], AF.Sqrt, [PB[3], B_misc], [Bsd[q]], bias=epsc, scale=1.0 / 64)
                recip(sdb[q], sdb[q], [Bsd[q]], [Bsd[q]])
                stt(dst[:, cs], banks[pb], gcol, sdb[q], ALU.mult, ALU.mult, [PB[pb], Bsd[q]] + CR, [Bdst])

            for tb in range(NB + 1):
                if tb < NB:
                    proj(tb)
                if tb >= 1:
                    fin(tb - 1)

        itc = [0]
        ipt = 0
        io = 0
        for h in range(8):
            s = h % 2
            load_wblk(wq_s[s], w_in_blk[h], Bwq[s])
            load_wblk(wk_s[s], w_in_blk[8 + h], Bwk[s])
            load_wblk(wv_s[s], w_in_blk[16 + h], Bwv[s])
            qk_proj(wk_s[s], Bwk[s], colt[:, 17:18], KT, B_KT, itc)
            qk_proj(wq_s[s], Bwq[s], gq8, Qall, B_Qall, itc)
            select_own(Qown.rearrange("p (n c) -> p n c", c=128),
                       Qall.rearrange("p (n two c) -> p n two c", two=2, c=128), B_Qall, B_Qown)
            for g4 in range(NT // 4):
                q = 4 + (g4 % 2)
                for tl in range(4):
                    t = g4 * 4 + tl
                    for kc in range(KC):
                        mm(banks[q][:, tl * 128:(tl + 1) * 128], hT_all[:, kc, t * 128:(t + 1) * 128], wv_s[s][:, kc, :],
                           kc == 0, kc == KC - 1, [B_hT, Bwv[s]], [PB[q]])
                cp(ACT if g4 % 2 == 0 else DVE, Vh[:, g4 * 4:g4 * 4 + 4, 0:128], banks[q].rearrange("p (a b) -> p a b", a=4),
                   [PB[q]], [B_Vh])
            items = []
            for i in range(NO):
                groups = []
                full = list(range(2 * i))
                for a in range(0, len(full), 4):
                    groups.append((full[a:a + 4], False))
                groups.append(([2 * i, 2 * i + 1], True))
                for gi_, (tiles, diag) in enumerate(groups):
                    for c in range(2):
                        items.append((i, c, tiles, diag, gi_ == 0, gi_ == len(groups) - 1))

            OBK = [5, 6, 7, 3]

            def emit_S(k):
                i, c, tiles, diag, fg, lg = items[k]
                sbank = k % 3
                for idx, jt in enumerate(tiles):
                    mm(banks[sbank][:, idx * 128:(idx + 1) * 128],
                       KT[c * 64:(c + 1) * 64, jt * 128:(jt + 1) * 128],
                       Qown[c * 64:(c + 1) * 64, i * 128:(i + 1) * 128], True, True, [B_KT, B_Qown], [PB[sbank]])

            def emit_rest(k):
                i, c, tiles, diag, fg, lg = items[k]
                sbank = k % 3
                pt = k % NPT
                n = len(tiles)
                ob_ = OBK[(i % 2) * 2 + c]
                act(pT[pt][:, 0:n * 128], banks[sbank][:, 0:n * 128], AF.Exp, [PB[sbank]], [BpT[pt]])
                if diag:
                    tt(DVE, pT[pt][:, 0:256], pT[pt][:, 0:256], maskb, ALU.mult, [BpT[pt], B_const], [BpT[pt]])
                for idx, jt in enumerate(tiles):
                    mm(banks[ob_][:, 0:130], pT[pt][:, idx * 128:(idx + 1) * 128], Vh[:, jt, :],
                       fg and idx == 0, lg and idx == n - 1, [BpT[pt], B_Vh], [PB[ob_]])
                return lg and c == 1

            def epilogue(i):
                oi = i % 2
                b0, b1 = OBK[oi * 2], OBK[oi * 2 + 1]
                st_ = ast[oi]
                recip(st_[:, 0:1], banks[b0][:, 128:129], [PB[b0]], [Bast[oi]])
                ts(DVE, o0[oi], banks[b0][:, 0:128], st_[:, 0:1], None, ALU.mult, None, [PB[b0], Bast[oi]], [Bo0[oi]])
                recip(st_[:, 1:2], banks[b1][:, 128:129], [PB[b1]], [Bast[oi]])
                tt(DVE, st_[:, 1:2], st_[:, 1:2], nlam, ALU.mult, [Bast[oi], B_misc], [Bast[oi]])
                stt(dif[oi], banks[b1][:, 0:128], st_[:, 1:2], o0[oi], ALU.mult, ALU.add,
                    [PB[b1], Bast[oi], Bo0[oi]], [Bdif[oi]])
                K.op(DVE, lambda: nc.vector.scalar_tensor_tensor(out=ajunk, in0=dif[oi], scalar=1.0, in1=dif[oi], op0=ALU.mult, op1=ALU.mult,
                                                                 accum_out=st_[:, 2:3]), [Bdif[oi]], [B_aj, Bast[oi]])
                ts(DVE, st_[:, 3:4], st_[:, 2:3], 1.0 / 128, EPS, ALU.mult, ALU.add, [Bast[oi]], [Bast[oi]])
                K.op(POOL, lambda: nc.gpsimd.tensor_tensor(out=st_[:, 4:5], in0=st_[:, 3:4], in1=mhalf, op=ALU.pow),
                     [Bast[oi], B_misc], [Bast[oi]])
                stt(dif[oi], dif[oi], st_[:, 4:5], sgrep, ALU.mult, ALU.mult, [Bdif[oi], Bast[oi], B_const], [Bdif[oi]])
                tr(banks[4][:, 0:128], dif[oi], [Bdif[oi]], [PB[4]])
                cp(DVE, attnT[:, h, i * 128:(i + 1) * 128], banks[4][:, 0:128], [PB[4]], [B_attnT])

            LA = 2
            DEF = 3
            pending = []
            for k in range(len(items) + LA):
                if k < len(items):
                    emit_S(k)
                if k >= LA:
                    if emit_rest(k - LA):
                        pending.append([DEF + 1, items[k - LA][0]])
                        convert_next()
                        convert_next()
                    for p_ in pending:
                        p_[0] -= 1
                    while pending and pending[0][0] <= 0:
                        epilogue(pending.pop(0)[1])
            for p_ in pending:
                epilogue(p_[1])
        while conv_state[0] < 2 * N_ET:
            convert_next()
        K.barrier()

        db = Bump(RD)
        wpat = db.take(BF16, [128, 8, 8, 128])
        B_wpa = Buf("wpa")
        for oc in range(8):
            K.dma(POOL, wpat[:, oc, :, :], wpa[oc], W=[B_wpa], own=B_wpa)
        eb = Bump(RE)
        hTo = [eb.take(BF16, [128, KC, 512]) for _ in range(2)]
        BhTo = [Buf("hTo0"), Buf("hTo1")]
        wga = [eb.take(BF16, [128, KC, 128]) for _ in range(2)]
        Bwga = [Buf("wga0"), Buf("wga1")]
        gsg = [eb.take(F32, [128, 512]) for _ in range(2)]
        Bgsg = [Buf("gsa0"), Buf("gsa1")]
        tmpm = [eb.take(F32, [128, 512]) for _ in range(2)]
        Btmpm = [Buf("tmpm0"), Buf("tmpm1")]
        def p4_a(n):
            ob, oc = divmod(n, 8)
            hs_ = ob % 2
            q = n % 2
            if oc == 0:
                for kc in range(KC):
                    select_own(hTo[hs_][:, kc, :].rearrange("p (n c) -> p n c", c=128),
                               hT_all[:, kc, ob * 1024:(ob + 1) * 1024].rearrange("p (n two c) -> p n two c", two=2, c=128),
                               B_hT, BhTo[hs_])
            cs = slice(ob * 512, (ob + 1) * 512)
            load_wblk(wga[q], w_in_blk[44 + oc], Bwga[q])
            for h in range(8):
                mm(banks[q], wpat[:, oc, h, :], attnT[:, h, cs], h == 0, h == 7, [B_wpa, B_attnT], [PB[q]])
            for kc in range(KC):
                mm(banks[2 + q], wga[q][:, kc, :], hTo[hs_][:, kc, :], kc == 0, kc == KC - 1, [Bwga[q], BhTo[hs_]], [PB[2 + q]])

        def p4_b(n):
            ob, oc = divmod(n, 8)
            q = n % 2
            cs = slice(ob * 512, (ob + 1) * 512)
            act(gsg[q], banks[2 + q], AF.Sigmoid, [PB[2 + q], B_const], [Bgsg[q]], bias=colt[:, oc:oc + 1])
            tt(DVE, tmpm[q], banks[q], gsg[q], ALU.mult, [PB[q], Bgsg[q]], [Btmpm[q]])
            tt(POOL, GR[:, oc, cs], GR[:, oc, cs], tmpm[q], ALU.add, [B_GR, Btmpm[q]], [B_GR])

        for n in range(NOB * 8 + 1):
            if n < NOB * 8:
                p4_a(n)
            if n >= 1:
                p4_b(n - 1)
        K.barrier()

        x2 = _view(RA, 0, F32, [128, NO, D])
        B_x2 = [Buf("x2_%d" % i) for i in range(NO)]
        db = Bump(RD)
        woutt = db.take(BF16, [128, KC, D])
        B_wout = Buf("wout")
        for kc in range(KC):
            K.dma(POOL, woutt[:, kc, :], wout[:, kc, :], W=[B_wout], own=B_wout)
        eb = Bump(RE)
        xo = [eb.take(F32, [128, D]) for _ in range(2)]
        Bxo = [Buf("xo0"), Buf("xo1")]
        for i in range(NO):
            s = i % 2
            K.dma(SP, xo[s], x_own[i * 128:(i + 1) * 128, :], W=[Bxo[s]], own=Bxo[s])
            for nb in range(2):
                q = 2 * s + nb
                for kc in range(KC):
                    mm(banks[q], GR[:, kc, i * 128:(i + 1) * 128], woutt[:, kc, nb * 512:(nb + 1) * 512], kc == 0, kc == KC - 1,
                       [B_GR, B_wout], [PB[q]])
                tt(DVE, x2[:, i, nb * 512:(nb + 1) * 512], banks[q], xo[s][:, nb * 512:(nb + 1) * 512], ALU.add,
                   [PB[q], Bxo[s]], [B_x2[i]])
        B_dbg = Buf("dbgst")
        if debug:
            for i in range(NO):
                K.dma(SP, dbg[i * 128:(i + 1) * 128, :], x2[:, i, :], R=[B_x2[i]], own=B_dbg)
        K.barrier()

        U32 = mybir.dt.uint32
        TB = 256
        NBLK = SO // TB
        CH = 8
        GT = _view(RBC, 0, BF16, [128, TB, 128])
        B_GT = Buf("GT")
        db = Bump(RD)
        h2Ts = [db.take(BF16, [128, KC, TB]) for _ in range(2)]
        B_h2Ts = [Buf("h2T0"), Buf("h2T1")]
        NU, NV = 4, 6
        Ut = [db.take(BF16, [128, KC, 128]) for _ in range(NU)]
        BUt = [Buf("Ut%d" % i) for i in range(NU)]
        Vt = [db.take(BF16, [128, D]) for _ in range(NV)]
        BVt = [Buf("Vt%d" % i) for i in range(NV)]
        GwT = [db.take(BF16, [128, EG, TB]) for _ in range(2)]
        BGw = [Buf("Gw0"), Buf("Gw1")]
        BGwR = [Buf("GwR%d" % i) for i in range(4)]
        geb = [db.take(BF16, [128, TB]) for _ in range(2)]
        Bge = [Buf("ge0"), Buf("ge1")]
        idxTs = [db.take(BF16, [128, 2, 128]) for _ in range(2)]
        B_idxTs = [Buf("idxT0"), Buf("idxT1")]
        ssb = [db.take(F32, [128, 256]) for _ in range(2)]
        Bssb = [Buf("ssb0"), Buf("ssb1")]
        iotab = banks[3][:, 256:384]
        B_iob = PB[3]
        cp(DVE, iotab, iotat, [B_const], [PB[3]])
        OH1 = [Vt[k].rearrange("p (c i) -> p c i", c=CH) for k in (0, 1)]
        OH2 = [Vt[k].rearrange("p (c i) -> p c i", c=CH) for k in (2, 3)]
        WF = [Vt[k].rearrange("p (c h r) -> p c h r", c=CH, h=8) for k in (4, 5)]
        BOH1, BOH2, BWF = [BVt[0], BVt[1]], [BVt[2], BVt[3]], [BVt[4], BVt[5]]
        Mt = [Ut[k][:, 0:4, :] for k in (0, 1)]
        BMt = [BUt[0], BUt[1]]
        eb = Bump(RE)
        keyst = eb.take(BF16, [128, 16, 128])
        B_keys = Buf("keys")
        K.dma(POOL, keyst, keysT[:, :, :], W=[B_keys], own=B_keys)
        wqs = [eb.take(BF16, [128, KC, 128]) for _ in range(2)]
        Bwqs = [Buf("wqs0"), Buf("wqs1")]
        qTb = [eb.take(BF16, [128, TB]) for _ in range(2)]
        BqTb = [Buf("qTb0"), Buf("qTb1")]
        pxn = eb.take(F32, [128, D])
        pstt = eb.take(F32, [128, 8])
        B_pscr = Buf("pscr")
        topv = [eb.take(F32, [128, 16, 16]) for _ in range(2)]
        idxu = [eb.take(U32, [128, 16, 16]) for _ in range(2)]
        B_top = [Buf("topv0"), Buf("topv1")]
        wk1 = eb.take(F32, [128, 128])
        B_wk1 = Buf("wk1")
        wk1s = [wk1, eb.take(F32, [128, 128])]
        B_wk1s = [B_wk1, Buf("wk1b")]
        cand = eb.take(F32, [128, 256])
        cwk = eb.take(F32, [128, 256])
        B_cand, B_cwk = Buf("cand"), Buf("cwk")
        ctop = eb.take(F32, [128, 8, 16])
        B_ctop = Buf("ctop")
        pst = eb.take(F32, [128, 64])
        B_pst = Buf("pst")
        ejunk = eb.take(F32, [128, 16])
        B_ej = Buf("ejunk")
        Fw = [eb.take(F32, [128, 256]) for _ in range(2)]
        BFw = [Buf("Fw0"), Buf("Fw1")]
        Wr = db.take(BF16, [128, 16, 128])
        B_Wr = Buf("Wr")
        idxf = eb.take(F32, [128, 2, 128])
        B_idxf = Buf("idxf")
        WTs = [eb.take(BF16, [128, 16, 128]) for _ in range(2)]
        B_WTs = [Buf("WT0"), Buf("WT1")]
        B_out = Buf("outst")
        g2c = colt[:, 20:28]

        def load_U(et):
            sl, e_ = et % NU, et % N_ET
            K.dma(SP, Ut[sl], u_bf[e_], R=conv_done, W=[BUt[sl]], own=BUt[sl])

        def load_V(et):
            sl, e_ = et % NV, et % N_ET
            K.dma(SP, Vt[sl], v_bf[e_ * 128:(e_ + 1) * 128, :], R=conv_done, W=[BVt[sl]], own=BVt[sl])

        def bf(bank):
            return banks[bank].bitcast(BF16)

        PB3q, PB3s = PB[3], PB[2]

        def prep(blk):
            h2T, B_h2T = h2Ts[blk % 2], B_h2Ts[blk % 2]
            for tl in range(2):
                i = blk * 2 + tl
                xt = x2[:, i, :]
                act(pxn, xt, AF.Square, [B_x2[i]], [B_pscr], accum=pstt[:, 0:1])
                ts(DVE, pstt[:, 1:2], pstt[:, 0:1], 1.0 / D, EPS, ALU.mult, ALU.add, [B_pscr], [B_pscr])
                act(pstt[:, 1:2], pstt[:, 1:2], AF.Sqrt, [B_pscr], [B_pscr])
                recip(pstt[:, 2:3], pstt[:, 1:2], [B_pscr], [B_pscr])
                ts(DVE, pxn, xt, pstt[:, 2:3], None, ALU.mult, None, [B_x2[i], B_pscr], [B_pscr])
                yield
                for half in range(2):
                    for q in range(4):
                        kc = half * 4 + q
                        tr(banks[2][:, q * 128:(q + 1) * 128], pxn[:, kc * 128:(kc + 1) * 128], [B_pscr], [PB[2]])
                    for q in range(4):
                        kc = half * 4 + q
                        dst = h2T[:, kc, tl * 128:(tl + 1) * 128]
                        src = banks[2][:, q * 128:(q + 1) * 128]
                        if q % 2:
                            ts(DVE, dst, src, g2c[:, kc:kc + 1], None, ALU.mult, None, [PB[2], B_const], [B_h2T])
                        else:
                            act(dst, src, AF.Copy, [PB[2], B_const], [B_h2T], scale=g2c[:, kc:kc + 1])
                    yield
            K.dma(POOL, wqs[0], wq[0], W=[Bwqs[0]], own=Bwqs[0])

            def q_part(hc):
                q = hc % 2
                if hc + 1 < 16:
                    K.dma(POOL, wqs[1 - q], wq[hc + 1], W=[Bwqs[1 - q]], own=Bwqs[1 - q])
                for kc in range(KC):
                    mm(banks[3][:, 0:TB], wqs[q][:, kc, :], h2T[:, kc, :], kc == 0, kc == KC - 1, [Bwqs[q], B_h2T], [PB3q])
                cp(ACT, qTb[q], banks[3][:, 0:TB], [PB3q], [BqTb[q]])

            def s_part(hc):
                q = hc % 2
                for t2 in range(2):
                    mm(banks[2][:, t2 * 128:(t2 + 1) * 128], qTb[q][:, t2 * 128:(t2 + 1) * 128], keyst[:, hc, :], True, True,
                       [BqTb[q], B_keys], [PB3s])
                cp(ACT, ssb[q], banks[2][:, 0:256], [PB3s], [Bssb[q]])
                sv = [ssb[q][:, t2 * 128:(t2 + 1) * 128] for t2 in range(2)]
                for t2 in range(2):
                    K.op(DVE, lambda t2=t2: nc.vector.max(out=topv[t2][:, hc, 0:8], in_=sv[t2]), [Bssb[q]], [B_top[t2]])
                for t2 in range(2):
                    K.op(DVE, lambda t2=t2: nc.vector.max_index(out=idxu[t2][:, hc, 0:8], in_max=topv[t2][:, hc, 0:8], in_values=sv[t2]),
                         [Bssb[q], B_top[t2]], [B_top[t2]])
                for t2 in range(2):
                    K.op(DVE, lambda t2=t2: nc.vector.match_replace(out=wk1s[t2], in_to_replace=topv[t2][:, hc, 0:8], in_values=sv[t2],
                                                                    imm_value=-1e30), [Bssb[q], B_top[t2]], [B_wk1s[t2]])
                for t2 in range(2):
                    K.op(DVE, lambda t2=t2: nc.vector.max(out=topv[t2][:, hc, 8:16], in_=wk1s[t2]), [B_wk1s[t2]], [B_top[t2]])
                for t2 in range(2):
                    K.op(DVE, lambda t2=t2: nc.vector.max_index(out=idxu[t2][:, hc, 8:16], in_max=topv[t2][:, hc, 8:16], in_values=wk1s[t2]),
                         [B_wk1s[t2], B_top[t2]], [B_top[t2]])

            for hc in range(17):
                if hc < 16:
                    q_part(hc)
                if hc >= 1:
                    s_part(hc - 1)
                    yield
            for tl in range(2):
                tv, iu, Bt = topv[tl], idxu[tl], B_top[tl]
                idxT, B_idxT, WT, B_WT = idxTs[tl], B_idxTs[tl], WTs[tl], B_WTs[tl]
                cand3 = cand.rearrange("p (a b) -> p a b", a=16)

                def mk_cand(h):
                    in0 = tv[:, 2 * h, :].unsqueeze(2).to_broadcast([128, 16, 16])
                    in1 = tv[:, 2 * h + 1, :].unsqueeze(1).to_broadcast([128, 16, 16])
                    tt(DVE, cand3, in0, in1, ALU.add, [Bt], [B_cand])
                for h in range(8):
                    mk_cand(h)
                    K.op(DVE, lambda h=h: nc.vector.max(out=ctop[:, h, 0:8], in_=cand), [B_cand], [B_ctop])
                    K.op(DVE, lambda h=h: nc.vector.match_replace(out=cwk, in_to_replace=ctop[:, h, 0:8], in_values=cand,
                                                                  imm_value=-1e30), [B_cand, B_ctop], [B_cwk])
                    K.op(DVE, lambda h=h: nc.vector.max(out=ctop[:, h, 8:16], in_=cwk), [B_cwk], [B_ctop])
                    yield
                ts(DVE, pst[:, 0:8], ctop[:, :, 0], -1.0, None, ALU.mult, None, [B_ctop], [B_pst])
                for h in range(8):
                    act(ejunk, ctop[:, h, :], AF.Exp, [B_ctop, B_pst], [B_ej, B_pst], bias=pst[:, h:h + 1], accum=pst[:, 8 + h:9 + h])
                act(pst[:, 16:24], pst[:, 8:16], AF.Ln, [B_pst], [B_pst])
                tt(DVE, pst[:, 24:32], pst[:, 0:8], pst[:, 16:24], ALU.subtract, [B_pst], [B_pst])
                ts(DVE, pst[:, 32:40], ctop[:, :, 15], -4e-6, None, ALU.add, None, [B_ctop, B_pst], [B_pst])
                tv4 = tv.rearrange("p (h c) r -> p h c r", c=2)
                E1 = cwk[:, 0:128].rearrange("p (h r) -> p h r", h=8)
                E2 = cwk[:, 128:256].rearrange("p (h r) -> p h r", h=8)
                act(E1, tv4[:, :, 0, :], AF.Exp, [Bt, B_cwk], [B_cwk])
                act(E2, tv4[:, :, 1, :], AF.Exp, [Bt, B_cwk], [B_cwk])
                act(pst[:, 40:48], pst[:, 24:32], AF.Exp, [B_pst], [B_pst])
                tt(DVE, E1, E1, pst[:, 40:48].unsqueeze(2).to_broadcast([128, 8, 16]), ALU.mult, [B_cwk, B_pst], [B_cwk])
                yield
                for h in range(8):
                    fq = h % 2
                    mk_cand(h)
                    fw3 = Fw[fq].rearrange("p (a b) -> p a b", a=16)
                    tt(DVE, fw3, E1[:, h, :].unsqueeze(2).to_broadcast([128, 16, 16]),
                       E2[:, h, :].unsqueeze(1).to_broadcast([128, 16, 16]), ALU.mult, [B_cwk], [BFw[fq]])
                    wout_v = Wr[:, :, h * 16:(h + 1) * 16].rearrange("p r2 r1 -> p r1 r2")
                    stt(wout_v, cand3, pst[:, 32 + h:33 + h], fw3, ALU.is_ge, ALU.mult,
                        [B_cand, B_pst, BFw[fq]], [B_Wr])
                    if h % 2 == 1:
                        yield
                iu4 = iu.rearrange("p (h c) r -> p h c r", c=2)
                for c in range(2):
                    cp(DVE, idxf[:, c, :].rearrange("p (h r) -> p h r", h=8), iu4[:, :, c, :], [Bt], [B_idxf])
                for c in range(2):
                    tr(banks[2][:, c * 128:(c + 1) * 128], idxf[:, c, :], [B_idxf], [PB[2]])
                cp(ACT, idxT.rearrange("p c t -> p (c t)"), banks[2][:, 0:256], [PB[2]], [B_idxT])
                yield
                for g8 in range(2):
                    for r8 in range(8):
                        r2 = g8 * 8 + r8
                        K.op(PE, lambda r2=r2, r8=r8: nc.tensor.transpose(bf(2)[:, r8 * 128:(r8 + 1) * 128], Wr[:, r2, :], identb),
                             [B_Wr, B_const], [PB[2]])
                    cp(ACT if g8 == 0 else DVE, WT[:, g8 * 8:(g8 + 1) * 8, :].rearrange("p r t -> p (r t)"), bf(2), [PB[2]], [B_WT])
                    yield

        def gbuild(blk):
            for tl in range(2):
                idxT, B_idxT, WT, B_WT = idxTs[tl], B_idxTs[tl], WTs[tl], B_WTs[tl]
                ngrp = 128 // 4

                def build_chunk(ch):
                    b_ = ch % 2
                    t0 = ch * CH
                    io_ = iotab.unsqueeze(1).to_broadcast([128, CH, 128])
                    tt(DVE, OH1[b_], io_, idxT[:, 0, t0:t0 + CH].unsqueeze(2).to_broadcast([128, CH, 128]), ALU.is_equal,
                       [B_iob, B_idxT], [BOH1[b_]])
                    tt(DVE, OH2[b_], io_, idxT[:, 1, t0:t0 + CH].unsqueeze(2).to_broadcast([128, CH, 128]), ALU.is_equal,
                       [B_iob, B_idxT], [BOH2[b_]])
                    w_in0 = WT[:, :, t0:t0 + CH].rearrange("p r t -> p t r").unsqueeze(2).to_broadcast([128, CH, 8, 16])
                    h_in1 = hmb.unsqueeze(1).unsqueeze(3).to_broadcast([128, CH, 8, 16])
                    tt(POOL, WF[b_], w_in0, h_in1, ALU.mult, [B_WT, B_const], [BWF[b_]])

                def emit1(g):
                    ch = (g * 4) // CH
                    if (g * 4) % CH == 0:
                        build_chunk(ch)
                    b_ = ch % 2
                    mq = g % 2
                    for k in range(4):
                        tk = (g * 4) % CH + k
                        mm(banks[mq][:, k * 128:(k + 1) * 128], WF[b_][:, tk, :, :].rearrange("p h r -> p (h r)"), OH1[b_][:, tk, :],
                           True, True, [BWF[b_], BOH1[b_]], [PB[mq]])
                    cp(ACT, Mt[mq].rearrange("p k i -> p (k i)"), banks[mq], [PB[mq]], [BMt[mq]])

                def emit2(g):
                    ch = (g * 4) // CH
                    b_ = ch % 2
                    mq = g % 2
                    for k in range(4):
                        tk = (g * 4) % CH + k
                        mm(banks[4 + mq][:, k * 128:(k + 1) * 128], OH2[b_][:, tk, :], Mt[mq][:, k, :], True, True,
                           [BOH2[b_], BMt[mq]], [PB[4 + mq]])
                    tg = tl * 128 + g * 4
                    cp(ACT, GT[:, tg:tg + 4, :].rearrange("p t i -> p (t i)"), banks[4 + mq], [PB[4 + mq]], [B_GT])

                for g in range(ngrp + 1):
                    if g < ngrp:
                        emit1(g)
                    if g >= 1:
                        emit2(g - 1)

        etc = 0
        for _ in prep(0):
            pass
        for blk in range(NBLK):
            h2T, B_h2T = h2Ts[blk % 2], B_h2Ts[blk % 2]
            gbuild(blk)
            nxt = prep(blk + 1) if blk + 1 < NBLK else iter(())
            load_U(etc)
            load_V(etc)
            load_U(etc + 1)
            load_V(etc + 1)
            load_U(etc + 2)

            def emit_A(g_et):
                us = g_et % NU
                aq = g_et % 2
                for kc in range(KC):
                    mm(banks[aq][:, 0:TB], Ut[us][:, kc, :], h2T[:, kc, :], kc == 0, kc == KC - 1, [BUt[us], B_h2T], [PB[aq]])

            emit_A(etc)
            emit_A(etc + 1)
            for et in range(N_ET):
                ge_ = etc
                if et + 3 < N_ET:
                    load_U(etc + 3)
                if et + 2 < N_ET:
                    load_V(etc + 2)
                aq = ge_ % 2
                if et % 2 == 0:
                    act(geb[0], banks[0][:, 0:TB], AF.Gelu_apprx_tanh, [PB[0]], [Bge[0]])
                    act(geb[1], banks[1][:, 0:TB], AF.Gelu_apprx_tanh, [PB[1]], [Bge[1]])
                    if et + 2 < N_ET:
                        emit_A(etc + 2)
                        emit_A(etc + 3)
                gs = ge_ % 4
                gw_t = GwT[gs // 2][:, (gs % 2) * 2:(gs % 2) * 2 + 1, :].rearrange("p a t -> p (a t)")
                tt(POOL, gw_t, GT[:, :, et], geb[aq], ALU.mult, [B_GT, Bge[aq]], [BGwR[gs]])
                vs = ge_ % NV
                for tl in range(2):
                    for nb in range(2):
                        oq = 4 + 2 * tl + nb
                        mm(banks[oq], gw_t[:, tl * 128:(tl + 1) * 128], Vt[vs][:, nb * 512:(nb + 1) * 512],
                           et == 0, et == N_ET - 1, [BGwR[gs], BVt[vs]], [PB[oq]])
                etc += 1
                next(nxt, None)
            for tl in range(2):
                i = blk * 2 + tl
                for nb in range(2):
                    oq = 4 + 2 * tl + nb
                    tt(DVE, x2[:, i, nb * 512:(nb + 1) * 512], banks[oq], x2[:, i, nb * 512:(nb + 1) * 512], ALU.add,
                       [PB[oq], B_x2[i]], [B_x2[i]])
            for _ in nxt:
                pass
            for tl in range(2):
                i = blk * 2 + tl
                K.dma(SP, out_own[i * 128:(i + 1) * 128, :], x2[:, i, :], R=[B_x2[i]], own=B_out)
        K.barrier()
    return nc


def _prep_shared(inp):
    f = np.float32
    w_in = np.asarray(inp["w_in"][0], f)
    sh = {}
    sh["w_in_blk"] = np.ascontiguousarray(w_in.reshape(KC, 128, 60, 128).transpose(2, 1, 0, 3))
    sh["g1"] = np.ascontiguousarray(inp["norm1_g"][0], f)
    sh["g2"] = np.ascontiguousarray(inp["norm2_g"][0], f)
    sh["lamv"] = np.ascontiguousarray(np.concatenate([inp["lambda_q1"][0], inp["lambda_k1"][0],
                                                      inp["lambda_q2"][0], inp["lambda_k2"][0]]), f)
    sh["sublng"] = np.ascontiguousarray(inp["subln_g"][0], f)
    rv = np.zeros((128, 10, 8), f)
    cw = np.asarray(inp["conv_w"][0], f)
    for tap in range(4):
        rv[:, :, tap] = cw[tap].reshape(10, 128).T
    rv[:, :, 4] = np.asarray(inp["conv_b"][0], f).reshape(10, 128).T
    rv[:, :, 5] = np.asarray(inp["b_rg_a"][0], f).reshape(10, 128).T
    rv[:, :, 6] = np.asarray(inp["b_rg_x"][0], f).reshape(10, 128).T
    rv[:, :, 7] = np.asarray(inp["rg_lambda"][0], f).reshape(10, 128).T
    sh["rnnvec"] = rv
    wbd = np.zeros((128, 10, 2, 128), f)
    wa = np.asarray(inp["w_rg_a"][0], f)
    wx = np.asarray(inp["w_rg_x"][0], f)
    for j in range(10):
        for half in range(2):
            sl = slice(half * 64, (half + 1) * 64)
            wbd[sl, j, 0, sl] = wa[2 * j + half]
            wbd[sl, j, 1, sl] = wx[2 * j + half]
    sh["wbd"] = wbd
    sh["wpa"] = np.ascontiguousarray(np.asarray(inp["w_br_attn"][0], f).reshape(8, 128, 8, 128).transpose(2, 1, 0, 3))
    sh["wpr"] = np.ascontiguousarray(np.asarray(inp["w_br_rnn"][0], f).reshape(10, 128, 8, 128).transpose(2, 1, 0, 3))
    sh["wout"] = np.ascontiguousarray(np.asarray(inp["w_out"][0], f).reshape(KC, 128, D).transpose(1, 0, 2))
    sh["wq"] = np.ascontiguousarray(np.asarray(inp["w_peer_q"][0], f).reshape(KC, 128, 16, 128).transpose(2, 1, 0, 3))
    sk = np.asarray(inp["peer_sub_keys"][0], f)
    sh["keysT"] = np.ascontiguousarray(sk.reshape(16, 128, 128).transpose(2, 0, 1))
    pu = np.asarray(inp["peer_u"][0], f)
    sh["u_t"] = np.ascontiguousarray(pu.reshape(N_ET, 128, KC, 128).transpose(0, 3, 2, 1))
    sh["v_tab"] = np.ascontiguousarray(inp["peer_v"][0], f)
    sh["ident"] = np.eye(128, dtype=f)
    bo = np.zeros((128, 128), f)
    bo[:64, :64] = 1
    bo[64:, 64:] = 1
    sh["bones"] = bo
    sh["iota"] = np.ascontiguousarray(np.broadcast_to(np.arange(128, dtype=f)[None, :], (128, 128)))
    hm = np.zeros((128, 8), f)
    hm[np.arange(128), np.arange(128) // 16] = 1.0
    sh["hm"] = hm
    return sh


def _prep_core(inp, sh, b, p, NT):
    f = np.float32
    m = dict(sh)
    x = np.asarray(inp["x"][b], f)[:NT * 128]
    m["x_all"] = np.ascontiguousarray(x)
    m["x_own"] = np.ascontiguousarray(x.reshape(NT // 2, 2, 128, D)[:, p].reshape(-1, D))
    cols = np.zeros((128, 32), f)
    cols[:, 0:16] = np.asarray(inp["b_gate"][0], f).reshape(16, 128).T
    cols[:, 16] = np.tile(np.asarray(inp["q_norm_g"][0], f), 2)
    cols[:, 17] = np.tile(np.asarray(inp["k_norm_g"][0], f), 2)
    cols[:, 18] = 1.0 if p == 0 else 0.0
    cols[:, 19] = 0.0 if p == 0 else 1.0
    cols[:, 20:28] = np.asarray(inp["norm2_g"][0], f).reshape(8, 128).T
    m["cols"] = cols
    k = np.arange(128)[:, None] // 64
    q = np.arange(128)[None, :] // 64
    dg = (k <= q).astype(f)
    if p == 0:
        m["mask2"] = np.concatenate([dg, np.zeros((128, 128), f)], axis=1)
    else:
        m["mask2"] = np.concatenate([np.ones((128, 128), f), dg], axis=1)
    return m


def run(inputs, NT=32, debug=False, n_batch=4):
    sh = _prep_shared(inputs)
    maps = []
    for c in range(2 * n_batch):
        maps.append(_prep_core(inputs, sh, c // 2, c % 2, NT))
    nc = build_nc(NT, debug=debug)
    res = run_bass_kernel_spmd(nc, maps, core_ids=list(range(2 * n_batch)))
    out = np.zeros((n_batch, NT * 128, D), np.float32)
    dbg = np.zeros_like(out) if debug else None
    for c in range(2 * n_batch):
        b, p = c // 2, c % 2
        r = res.results[c]
        out[b].reshape(NT // 2, 2, 128, D)[:, p] = np.asarray(r["out_own"]).reshape(NT // 2, 128, D)
        if debug:
            dbg[b].reshape(NT // 2, 2, 128, D)[:, p] = np.asarray(r["dbg_x2"]).reshape(NT // 2, 128, D)
    return (out, dbg) if debug else out


def kernel(**inputs):
    return run(inputs, NT=32, debug=False, n_batch=4)
```

```python
import contextlib
import numpy as np
import concourse.bass as bass
import concourse.mybir as mybir
from concourse.bass_utils import run_bass_kernel_spmd

F32 = mybir.dt.float32
BF16 = mybir.dt.bfloat16
AF = mybir.ActivationFunctionType
ALU = mybir.AluOpType

D = 1024
KC = 8
EPS = 1e-6
LAM_INIT = 0.2
N_ET = 128
EG = 4


class Tok:
    __slots__ = ("key", "sem", "val")

    def __init__(self, key, sem, val):
        self.key, self.sem, self.val = key, sem, val


class Buf:
    def __init__(self, name):
        self.name = name
        self.w = None
        self.r = {}
        self.dsem = {}
        self.dcnt = {}


class Eng:
    def __init__(self, K, eng, name, is_pe=False, counted=True):
        self.K, self.eng, self.name, self.is_pe, self.counted = K, eng, name, is_pe, counted
        self.epoch = 0
        self.count = 0
        self.seen = {}
        self.sem = K.new_sem(name + "0") if counted else None

    @property
    def key(self):
        return "%s@%d" % (self.name, self.epoch)

    def bump(self):
        if self.count >= 20000:
            self.epoch += 1
            self.count = 0
            self.sem = self.K.new_sem("%s%d" % (self.name, self.epoch))


class Kern:
    def __init__(self, nc, es):
        self.nc, self.es = nc, es
        self.nsem = 0
        self.pe = Eng(self, nc.tensor, "pe", is_pe=True)
        self.act = Eng(self, nc.scalar, "act")
        self.dve = Eng(self, nc.vector, "dve")
        self.pool = Eng(self, nc.gpsimd, "pool")
        self.sp = Eng(self, nc.sync, "sp", counted=False)
        self.engs = [self.pe, self.act, self.dve, self.pool, self.sp]
        self.dma_toks = {}

    def new_sem(self, name):
        self.nsem += 1
        return self.es.enter_context(self.nc.semaphore("s_%s_%d" % (name, self.nsem)))

    def _deps(self, E, R, W):
        deps = {}

        def add(t):
            if t is None:
                return
            o = deps.get(t.key)
            if o is None or o.val < t.val:
                deps[t.key] = t
        for b in R:
            add(b.w)
        for b in W:
            add(b.w)
            for t in b.r.values():
                add(t)
        for key, t in deps.items():
            if E.is_pe and key.startswith("pe@"):
                continue
            if E.seen.get(key, 0) < t.val:
                E.eng.wait_ge(t.sem, t.val)
                E.seen[key] = t.val

    def op(self, E, fn, R=(), W=()):
        self._deps(E, R, W)
        ins = fn()
        E.bump()
        E.count += 1
        ins.then_inc(E.sem, 1)
        tok = Tok(E.key, E.sem, E.count)
        for b in R:
            b.r[tok.key] = tok
        for b in W:
            b.w = tok
            b.r = {}
        return ins

    def dma(self, Q, out, in_, R=(), W=(), own=None):
        self._deps(Q, R, W)
        b = own
        qn = Q.name
        if qn not in b.dsem:
            b.dsem[qn] = self.new_sem("d")
            b.dcnt[qn] = 0
        b.dcnt[qn] += 1
        Q.eng.dma_start(out=out, in_=in_).then_inc(b.dsem[qn], 16)
        tok = Tok("d:" + b.name + ":" + qn, b.dsem[qn], 16 * b.dcnt[qn])
        self.dma_toks[tok.key] = tok
        for x in R:
            x.r[tok.key] = tok
        for x in W:
            x.w = tok
            x.r = {}

    def barrier(self):
        toks = []
        for P in (self.pe, self.act, self.dve, self.pool):
            if P.count > 0:
                toks.append(Tok(P.key, P.sem, P.count))
        toks += list(self.dma_toks.values())
        for E in self.engs:
            for t in toks:
                if E.seen.get(t.key, 0) < t.val:
                    E.eng.wait_ge(t.sem, t.val)
                    E.seen[t.key] = t.val


def _view(reg, boff, dt, shape):
    esz = 2 if dt == BF16 else 4
    n = 1
    for s in shape[1:]:
        n *= s
    nb = n * esz
    assert boff % 4 == 0 and nb % 4 == 0
    assert boff + nb <= reg.shape[1] * 4, ("region overflow", boff, nb, reg.shape)
    v = reg[:, boff // 4:(boff + nb) // 4]
    if dt != F32:
        v = v.bitcast(dt)
    fs = shape[1:]
    if len(fs) == 2:
        v = v.rearrange("p (a b) -> p a b", a=fs[0])
    elif len(fs) == 3:
        v = v.rearrange("p (a b c) -> p a b c", a=fs[0], b=fs[1])
    elif len(fs) == 4:
        v = v.rearrange("p (a b c d) -> p a b c d", a=fs[0], b=fs[1], c=fs[2])
    return v


class Bump:
    def __init__(self, reg):
        self.reg = reg
        self.off = 0

    def reset(self):
        self.off = 0

    def take(self, dt, shape):
        esz = 2 if dt == BF16 else 4
        n = 1
        for s in shape[1:]:
            n *= s
        nb = (n * esz + 31) // 32 * 32
        v = _view(self.reg, self.off, dt, shape) if (n * esz) % 4 == 0 else None
        self.off += nb
        return v


def build_nc(NT, debug=False):
    NO = NT // 2
    S = NT * 128
    SO = NO * 128
    NB = S // 512
    NOB = SO // 512
    assert S % 512 == 0 and SO % 512 == 0

    nc = bass.Bass("TRN2", target_bir_lowering=False)

    def din(name, shape, dt=F32):
        return nc.dram_tensor(name, list(shape), dt, kind="ExternalInput").ap()

    x_all = din("x_all", [S, D])
    x_own = din("x_own", [SO, D])
    w_in_blk = din("w_in_blk", [60, 128, KC, 128])
    g1 = din("g1", [D])
    g2 = din("g2", [D])
    cols = din("cols", [128, 32])
    lamv = din("lamv", [256])
    sublng = din("sublng", [128])
    rnnvec = din("rnnvec", [128, 10, 8])
    wbd = din("wbd", [128, 10, 2, 128])
    wpa = din("wpa", [8, 128, 8, 128])
    wpr = din("wpr", [8, 128, 10, 128])
    wout = din("wout", [128, KC, D])
    wq = din("wq", [16, 128, KC, 128])
    keysT = din("keysT", [128, 16, 128])
    u_t = din("u_t", [N_ET, 128, KC, 128])
    v_tab = din("v_tab", [N_ET * 128, D])
    mask2 = din("mask2", [128, 256])
    ident_d = din("ident", [128, 128])
    bones_d = din("bones", [128, 128])
    iota_d = din("iota", [128, 128])
    hm_d = din("hm", [128, 8])
    out_own = nc.dram_tensor("out_own", [SO, D], F32, kind="ExternalOutput").ap()
    u_bf = nc.dram_tensor("u_bf16", [N_ET, 128, KC, 128], BF16, kind="Internal").ap()
    v_bf = nc.dram_tensor("v_bf16", [N_ET * 128, D], BF16, kind="Internal").ap()
    dbg = None
    if debug:
        dbg = nc.dram_tensor("dbg_x2", [SO, D], F32, kind="ExternalOutput").ap()

    with contextlib.ExitStack() as es:
        K = Kern(nc, es)
        PE, ACT, DVE, POOL, SP = K.pe, K.act, K.dve, K.pool, K.sp

        def sb(name, n_f32):
            return es.enter_context(nc.sbuf_tensor(name, [128, n_f32], F32))[:, :]

        R0 = sb("R0", 32768)
        RA = R0[:, 0:16384]
        RB = R0[:, 16384:24576]
        RC = R0[:, 24576:32768]
        RBC = R0[:, 16384:32768]
        RD = sb("RD", 10240)
        RE = sb("RE", 8192)
        CT = sb("CT", 1280)
        banks = [es.enter_context(nc.psum_tensor("ps%d" % i, [128, 512], F32))[:, :] for i in range(8)]
        PB = [Buf("ps%d" % i) for i in range(8)]

        def mm(out, lhsT, rhs, start, stop, R, W):
            K.op(PE, lambda: nc.tensor.matmul(out, lhsT=lhsT, rhs=rhs, start=start, stop=stop), R, W)

        def tr(out, in_, R, W):
            K.op(PE, lambda: nc.tensor.transpose(out, in_, ident), R + [B_const], W)

        def act(out, in_, func, R, W, bias=None, scale=None, accum=None):
            kw = {}
            if bias is not None:
                kw["bias"] = bias
            if scale is not None:
                kw["scale"] = scale
            if accum is not None:
                kw["accum_out"] = accum
            K.op(ACT, lambda: nc.scalar.activation(out=out, in_=in_, func=func, **kw), R, W)

        def ts(E, out, in0, s1, s2, op0, op1, R, W):
            if op1 is None:
                K.op(E, lambda: E.eng.tensor_scalar(out=out, in0=in0, scalar1=s1, scalar2=None, op0=op0), R, W)
            else:
                K.op(E, lambda: E.eng.tensor_scalar(out=out, in0=in0, scalar1=s1, scalar2=s2, op0=op0, op1=op1), R, W)

        def stt(out, in0, scalar, in1, op0, op1, R, W):
            K.op(DVE, lambda: nc.vector.scalar_tensor_tensor(out=out, in0=in0, scalar=scalar, in1=in1, op0=op0, op1=op1), R, W)

        def tt(E, out, in0, in1, op, R, W):
            K.op(E, lambda: E.eng.tensor_tensor(out=out, in0=in0, in1=in1, op=op), R, W)

        def cp(E, out, in_, R, W):
            if E is ACT:
                K.op(E, lambda: nc.scalar.copy(out=out, in_=in_), R, W)
            else:
                K.op(E, lambda: E.eng.tensor_copy(out=out, in_=in_), R, W)

        def recip(out, in_, R, W):
            K.op(DVE, lambda: nc.vector.reciprocal(out=out, in_=in_), R, W)

        def memset(E, ap, val, W):
            K.op(E, lambda: E.eng.memset(ap, val), [], W)

        cb = Bump(CT)
        ident = cb.take(F32, [128, 128])
        identb = cb.take(BF16, [128, 128])
        bonesb = cb.take(BF16, [128, 128])
        maskb = cb.take(BF16, [128, 256])
        colt = cb.take(F32, [128, 32])
        rnv = cb.take(F32, [128, 10, 8])
        sgrep = cb.take(F32, [128, 128])
        lamt = cb.take(F32, [128, 256])
        misc = cb.take(F32, [128, 64])
        iotat = cb.take(F32, [128, 128])
        rn2 = cb.take(F32, [128, 40])
        B_rn2 = Buf("rn2")
        hmb = cb.take(BF16, [128, 8])
        B_const = Buf("const")
        K.dma(SP, ident, ident_d[:, :], W=[B_const], own=B_const)
        K.dma(POOL, identb, ident_d[:, :], W=[B_const], own=B_const)
        K.dma(POOL, bonesb, bones_d[:, :], W=[B_const], own=B_const)
        K.dma(POOL, maskb, mask2[:, :], W=[B_const], own=B_const)
        K.dma(SP, colt, cols[:, :], W=[B_const], own=B_const)
        K.dma(SP, iotat, iota_d[:, :], W=[B_const], own=B_const)
        K.dma(POOL, hmb, hm_d[:, :], W=[B_const], own=B_const)
        K.dma(SP, rnv, rnnvec[:, :, :], W=[B_const], own=B_const)
        K.dma(SP, sgrep, sublng.partition_broadcast(128), W=[B_const], own=B_const)
        K.dma(SP, lamt, lamv.partition_broadcast(128), W=[B_const], own=B_const)
        B_misc = Buf("misc")
        epsc = misc[:, 0:1]
        gq8 = misc[:, 1:2]
        nlam = misc[:, 2:3]
        sa = misc[:, 8:18]
        sa2 = misc[:, 18:28]
        memset(DVE, misc[:, 0:1], EPS, [B_misc])
        mhalf = misc[:, 30:31]
        memset(DVE, misc[:, 30:31], -0.5, [B_misc])
        ts(DVE, gq8, colt[:, 16:17], 0.125, None, ALU.mult, None, [B_const, B_misc], [B_misc])
        junk64 = misc[:, 32:96] if False else None
        lw = lamt
        tt(DVE, lamt[:, 0:64], lamt[:, 0:64], lamt[:, 64:128], ALU.mult, [B_const], [B_const])
        tt(DVE, lamt[:, 128:192], lamt[:, 128:192], lamt[:, 192:256], ALU.mult, [B_const], [B_const])
        K.op(DVE, lambda: nc.vector.reduce_sum(out=misc[:, 3:4], in_=lamt[:, 0:64], axis=mybir.AxisListType.X), [B_const, B_misc], [B_misc])
        K.op(DVE, lambda: nc.vector.reduce_sum(out=misc[:, 4:5], in_=lamt[:, 128:192], axis=mybir.AxisListType.X), [B_const, B_misc], [B_misc])
        act(misc[:, 3:5], misc[:, 3:5], AF.Exp, [B_misc], [B_misc])
        tt(DVE, misc[:, 5:6], misc[:, 4:5], misc[:, 3:4], ALU.subtract, [B_misc], [B_misc])
        ts(DVE, nlam, misc[:, 5:6], -LAM_INIT, None, ALU.add, None, [B_misc], [B_misc])
        act(sa, rnv[:, :, 7], AF.Exp, [B_const, B_misc], [B_misc], scale=-1.0)
        ts(DVE, sa, sa, 1.0, None, ALU.add, None, [B_misc], [B_misc])
        act(sa, sa, AF.Ln, [B_misc], [B_misc])
        ts(DVE, sa2, sa, -16.0, None, ALU.mult, None, [B_misc], [B_misc])
        ts(DVE, sa, sa, -8.0, None, ALU.mult, None, [B_misc], [B_misc])
        ts(DVE, sgrep, sgrep, 1.0 - LAM_INIT, None, ALU.mult, None, [B_const], [B_const])
        CR = [B_const, B_misc]

        def load_wblk(dst, src_ap, buf):
            K.dma(POOL, dst, src_ap, W=[buf], own=buf)

        def norm_T(xt, Bx, grep, Bg, scratch, Bs, dst_fn, Bdst, pb0, pb1):
            act(scratch["junk"], xt, AF.Square, [Bx], [Bs], accum=scratch["ss"])
            ts(DVE, scratch["ms"], scratch["ss"], 1.0 / D, EPS, ALU.mult, ALU.add, [Bs], [Bs])
            act(scratch["ms"], scratch["ms"], AF.Sqrt, [Bs], [Bs])
            recip(scratch["rs"], scratch["ms"], [Bs], [Bs])
            stt(scratch["xn"], xt, scratch["rs"], grep, ALU.mult, ALU.mult, [Bx, Bg, Bs], [Bs])
            for half, pb in ((0, pb0), (1, pb1)):
                for q in range(4):
                    kc = half * 4 + q
                    tr(banks[pb][:, q * 128:(q + 1) * 128], scratch["xn"][:, kc * 128:(kc + 1) * 128], [Bs], [PB[pb]])
                E = ACT if half == 0 else DVE
                cp(E, dst_fn(half), banks[pb].rearrange("p (a b) -> p a b", a=4), [PB[pb]], [Bdst])

        def select_own(dst, src_pairs, Bsrc, Bdst):
            ts(DVE, dst, src_pairs[:, :, 0, :], colt[:, 18:19], None, ALU.mult, None, [Bsrc, B_const], [Bdst])
            stt(dst, src_pairs[:, :, 1, :], colt[:, 19:20], dst, ALU.mult, ALU.add, [Bsrc, B_const, Bdst], [Bdst])

        hT_all = _view(RA, 0, BF16, [128, KC, S])
        B_hT = Buf("hT_all")
        eb = Bump(RE)
        g1rep = eb.take(F32, [128, D])
        B_g1 = Buf("g1rep")
        K.dma(SP, g1rep, g1.partition_broadcast(128), W=[B_g1], own=B_g1)
        xs = [eb.take(F32, [128, D]) for _ in range(2)]
        Bxs = [Buf("xs0"), Buf("xs1")]
        scr = []
        Bscr = []
        for i in range(2):
            scr.append({"xn": eb.take(F32, [128, D]), "junk": eb.take(BF16, [128, D]), "st": eb.take(F32, [128, 8])})
            scr[i]["ss"] = scr[i]["st"][:, 0:1]
            scr[i]["ms"] = scr[i]["st"][:, 1:2]
            scr[i]["rs"] = scr[i]["st"][:, 2:3]
            Bscr.append(Buf("scr%d" % i))
        def p1_a(t):
            s_ = t % 2
            sc_, Bs_ = scr[s_], Bscr[s_]
            K.dma(SP, xs[s_], x_all[t * 128:(t + 1) * 128, :], W=[Bxs[s_]], own=Bxs[s_])
            act(sc_["junk"], xs[s_], AF.Square, [Bxs[s_]], [Bs_], accum=sc_["ss"])
            ts(DVE, sc_["ms"], sc_["ss"], 1.0 / D, EPS, ALU.mult, ALU.add, [Bs_], [Bs_])
            act(sc_["ms"], sc_["ms"], AF.Sqrt, [Bs_], [Bs_])
            recip(sc_["rs"], sc_["ms"], [Bs_], [Bs_])
            stt(sc_["xn"], xs[s_], sc_["rs"], g1rep, ALU.mult, ALU.mult, [Bxs[s_], B_g1, Bs_], [Bs_])

        def p1_b(t):
            s_ = t % 2
            sc_, Bs_ = scr[s_], Bscr[s_]
            for half in range(2):
                pb = 2 * s_ + half
                for q in range(4):
                    kc = half * 4 + q
                    tr(banks[pb][:, q * 128:(q + 1) * 128], sc_["xn"][:, kc * 128:(kc + 1) * 128], [Bs_], [PB[pb]])
                cp(ACT if half == 0 else DVE, hT_all[:, half * 4:half * 4 + 4, t * 128:(t + 1) * 128],
                   banks[pb].rearrange("p (a b) -> p a b", a=4), [PB[pb]], [B_hT])

        for t in range(NT + 1):
            if t < NT:
                p1_a(t)
            if t >= 1:
                p1_b(t - 1)
        K.barrier()

        hT_own = _view(RB, 0, BF16, [128, KC, SO])
        B_hTo = Buf("hT_own")
        rnnT = _view(RD, 0, BF16, [128, 10, SO])
        B_rnnT = Buf("rnnT")
        GR = _view(RC, 0, BF16, [128, 8, SO])
        B_GR = Buf("GR")
        for kc in range(KC):
            select_own(hT_own[:, kc, :].rearrange("p (n c) -> p n c", c=128),
                       hT_all[:, kc, :].rearrange("p (n two c) -> p n two c", two=2, c=128), B_hT, B_hTo)
        eb = Bump(RE)
        wxr = [eb.take(BF16, [128, KC, 128]) for _ in range(2)]
        wyr = [eb.take(BF16, [128, KC, 128]) for _ in range(2)]
        Bwxr = [Buf("wxr0"), Buf("wxr1")]
        Bwyr = [Buf("wyr0"), Buf("wyr1")]
        wbdt = eb.take(BF16, [128, 10, 2, 128])
        B_wbd = Buf("wbd")
        K.dma(POOL, wbdt, wbd[:, :, :, :], W=[B_wbd], own=B_wbd)
        NRB = 2
        rb = []
        eb2 = Bump(RC)
        for i in range(NRB):
            if i == 1:
                eb = eb2
            d = {"xr": eb.take(F32, [128, 516]), "xc": eb.take(F32, [128, 512]), "xcb": eb.take(BF16, [128, 512]),
                 "gr": eb.take(F32, [128, 512]), "gi": eb.take(F32, [128, 512]), "a": eb.take(F32, [128, 512]),
                 "m": eb.take(F32, [128, 512]), "h": eb.take(F32, [128, 512]),
                 "gy": eb.take(F32, [128, 256]), "hs": eb.take(F32, [128, 256])}
            d["B"] = {k: Buf("rb%d_%s" % (i, k)) for k in ("xr", "xc", "xcb", "gr", "gi", "a", "m", "h", "gy", "hs")}
            rb.append(d)
        zcol = misc[:, 29:30]
        memset(DVE, misc[:, 29:30], 0.0, [B_misc])
        halfc = misc[:, 31:32]
        memset(DVE, misc[:, 31:32], 0.5, [B_misc])
        q25c = misc[:, 6:7]
        memset(DVE, misc[:, 6:7], 0.25, [B_misc])
        ts(DVE, rn2[:, 0:10], rnv[:, :, 5], 0.5, None, ALU.mult, None, [B_const], [B_rn2])
        ts(DVE, rn2[:, 10:20], rnv[:, :, 6], 0.5, None, ALU.mult, None, [B_const], [B_rn2])
        ts(DVE, rn2[:, 20:30], sa, 0.5, None, ALU.mult, None, [B_misc], [B_rn2])
        ts(DVE, rn2[:, 30:40], sa2, 0.5, None, ALU.mult, None, [B_misc], [B_rn2])
        GK, GC = 0.7978845608028654, 0.044715
        its = [(j, tb) for j in range(10) for tb in range(NB)]

        def stage_a(n):
            j, tb = its[n]
            s = j % 2
            if tb == 0:
                load_wblk(wxr[s], w_in_blk[24 + j], Bwxr[s])
                load_wblk(wyr[s], w_in_blk[34 + j], Bwyr[s])
            r, rp = rb[n % NRB], rb[(n - 1) % NRB]
            B = r["B"]
            pbx = n % 2
            for kc in range(KC):
                mm(banks[pbx], wxr[s][:, kc, :], hT_all[:, kc, tb * 512:(tb + 1) * 512], kc == 0, kc == KC - 1,
                   [Bwxr[s], B_hT], [PB[pbx]])
            if tb == 0:
                memset(DVE, r["xr"][:, 0:3], 0.0, [B["xr"]])
            else:
                cp(DVE, r["xr"][:, 0:3], rp["xr"][:, 512:515], [rp["B"]["xr"]], [B["xr"]])
            cp(ACT, r["xr"][:, 3:515], banks[pbx], [PB[pbx]], [B["xr"]])
            ts(POOL, r["xc"], r["xr"][:, 0:512], rnv[:, j, 0:1], rnv[:, j, 4:5], ALU.mult, ALU.add, [B["xr"], B_const], [B["xc"]])
            for tap in (1, 2, 3):
                stt(r["xc"], r["xr"][:, tap:tap + 512], rnv[:, j, tap:tap + 1], r["xc"], ALU.mult, ALU.add,
                    [B["xr"], B_const, B["xc"]], [B["xc"]])
            cp(POOL, r["xcb"], r["xc"], [B["xc"]], [B["xcb"]])
            oc0 = tb * 256
            for kc in range(KC):
                mm(banks[6 + pbx][:, 0:256], wyr[s][:, kc, :], hT_own[:, kc, oc0:oc0 + 256], kc == 0, kc == KC - 1,
                   [Bwyr[s], B_hTo], [PB[6 + pbx]])

        def stage_a2(n):
            j, tb = its[n]
            r = rb[n % NRB]
            B = r["B"]
            pbx = n % 2
            mm(banks[2 + pbx], wbdt[:, j, 0, :], r["xcb"], True, True, [B_wbd, B["xcb"]], [PB[2 + pbx]])
            mm(banks[4 + pbx], wbdt[:, j, 1, :], r["xcb"], True, True, [B_wbd, B["xcb"]], [PB[4 + pbx]])

        def stage_b(n):
            j, tb = its[n]
            r, rp = rb[n % NRB], rb[(n - 1) % NRB]
            B = r["B"]
            pbx = n % 2
            RN = [B_rn2]
            act(r["gr"], banks[2 + pbx], AF.Tanh, [PB[2 + pbx]] + RN, [B["gr"]], bias=rn2[:, j:j + 1], scale=0.5)
            act(r["gi"], banks[4 + pbx], AF.Tanh, [PB[4 + pbx]] + RN, [B["gi"]], bias=rn2[:, 10 + j:11 + j], scale=0.5)
            act(r["a"], r["gr"], AF.Exp, [B["gr"]] + RN, [B["a"]], scale=rn2[:, 20 + j:21 + j], bias=rn2[:, 20 + j:21 + j])
            act(r["m"], r["gr"], AF.Exp, [B["gr"]] + RN, [B["m"]], scale=rn2[:, 30 + j:31 + j], bias=rn2[:, 30 + j:31 + j])
            act(r["m"], r["m"], AF.Sqrt, [B["m"], B_misc], [B["m"]], scale=-0.25, bias=q25c)
            stt(r["gi"], r["gi"], 1.0, r["xc"], ALU.add, ALU.mult, [B["gi"], B["xc"]], [B["gi"]])
            tt(DVE, r["gi"], r["gi"], r["m"], ALU.mult, [B["gi"], B["m"]], [B["gi"]])
            init = zcol if tb == 0 else rp["h"][:, 511:512]
            RI = [B_misc] if tb == 0 else [rp["B"]["h"]]
            K.op(DVE, lambda: nc.vector.tensor_tensor_scan(out=r["h"], data0=r["a"], data1=r["gi"], initial=init,
                                                           op0=ALU.mult, op1=ALU.add),
                 [B["a"], B["gi"]] + RI, [B["h"]])
            yps = banks[6 + pbx][:, 0:256]
            act(r["gy"], yps, AF.Square, [PB[6 + pbx]], [B["gy"]])
            ts(DVE, r["gy"], r["gy"], GC, 1.0, ALU.mult, ALU.add, [B["gy"]], [B["gy"]])
            tt(DVE, r["gy"], r["gy"], yps, ALU.mult, [B["gy"], PB[6 + pbx]], [B["gy"]])
            act(r["gy"], r["gy"], AF.Tanh, [B["gy"]], [B["gy"]], scale=GK)
            stt(r["gy"], r["gy"], 1.0, yps, ALU.add, ALU.mult, [B["gy"], PB[6 + pbx]], [B["gy"]])
            select_own(r["hs"].rearrange("p (n c) -> p n c", c=128),
                       r["h"].rearrange("p (n two c) -> p n two c", two=2, c=128), B["h"], B["hs"])
            oc0 = tb * 256
            stt(rnnT[:, j, oc0:oc0 + 256], r["hs"], 0.5, r["gy"], ALU.mult, ALU.mult, [B["hs"], B["gy"]], [B_rnnT])

        stage_a(0)
        for n in range(len(its)):
            if n + 1 < len(its):
                stage_a(n + 1)
            stage_a2(n)
            stage_b(n)
        K.barrier()
        eb = Bump(RE)
        wprs = [eb.take(BF16, [128, 10, 128]) for _ in range(2)]
        wgs = [eb.take(BF16, [128, KC, 128]) for _ in range(2)]
        Bwprs = [Buf("wpr0"), Buf("wpr1")]
        Bwgs = [Buf("wg0"), Buf("wg1")]
        gsg = [eb.take(F32, [128, 512]) for _ in range(2)]
        Bgsg = [Buf("gsg0"), Buf("gsg1")]
        def p2_a(n):
            oc, ob = divmod(n, NOB)
            s_ = oc % 2
            q = n % 2
            if ob == 0:
                K.dma(POOL, wprs[s_], wpr[oc], W=[Bwprs[s_]], own=Bwprs[s_])
                load_wblk(wgs[s_], w_in_blk[52 + oc], Bwgs[s_])
            cs = slice(ob * 512, (ob + 1) * 512)
            for j in range(10):
                mm(banks[q], wprs[s_][:, j, :], rnnT[:, j, cs], j == 0, j == 9, [Bwprs[s_], B_rnnT], [PB[q]])
            for kc in range(KC):
                mm(banks[2 + q], wgs[s_][:, kc, :], hT_own[:, kc, cs], kc == 0, kc == KC - 1, [Bwgs[s_], B_hTo], [PB[2 + q]])

        def p2_b(n):
            oc, ob = divmod(n, NOB)
            q = n % 2
            cs = slice(ob * 512, (ob + 1) * 512)
            act(gsg[q], banks[2 + q], AF.Sigmoid, [PB[2 + q], B_const], [Bgsg[q]], bias=colt[:, 8 + oc:9 + oc])
            tt(DVE, GR[:, oc, cs], banks[q], gsg[q], ALU.mult, [PB[q], Bgsg[q]], [B_GR])

        for n in range(8 * NOB + 1):
            if n < 8 * NOB:
                p2_a(n)
            if n >= 1:
                p2_b(n - 1)
        K.barrier()

        attnT = _view(RB, 0, BF16, [128, 8, SO])
        B_attnT = Buf("attnT")
        db = Bump(RD)
        KT = db.take(BF16, [128, S])
        Qall = db.take(BF16, [128, S])
        Qown = db.take(BF16, [128, SO])
        Vh = db.take(BF16, [128, NT, 130])
        B_KT, B_Qall, B_Qown, B_Vh = Buf("KT"), Buf("Qall"), Buf("Qown"), Buf("Vh")
        memset(DVE, Vh[:, :, 128:130], 1.0, [B_Vh])
        eb = Bump(RE)
        wq_s = [eb.take(BF16, [128, KC, 128]) for _ in range(2)]
        wk_s = [eb.take(BF16, [128, KC, 128]) for _ in range(2)]
        wv_s = [eb.take(BF16, [128, KC, 128]) for _ in range(2)]
        Bwq = [Buf("wq0"), Buf("wq1")]
        Bwk = [Buf("wk0"), Buf("wk1")]
        Bwv = [Buf("wv0"), Buf("wv1")]
        sqb = [eb.take(BF16, [128, 512]) for _ in range(2)]
        sdb = [eb.take(F32, [128, 512]) for _ in range(2)]
        Bsq = [Buf("sq0"), Buf("sq1")]
        Bsd = [Buf("sd0"), Buf("sd1")]
        NPT = 4
        pT = [eb.take(BF16, [128, 512]) for _ in range(NPT)]
        BpT = [Buf("pT%d" % i) for i in range(NPT)]
        o0 = [eb.take(F32, [128, 128]) for _ in range(2)]
        dif = [eb.take(F32, [128, 128]) for _ in range(2)]
        Bo0 = [Buf("o00"), Buf("o01")]
        Bdif = [Buf("dif0"), Buf("dif1")]
        ast = [eb.take(F32, [128, 8]) for _ in range(2)]
        Bast = [Buf("ast0"), Buf("ast1")]
        ajunk = eb.take(BF16, [128, 128])
        B_aj = Buf("ajunk")
        NSTG = 3
        stg = [eb.take(BF16, [128, D]) for _ in range(NSTG)]
        Bstg = [Buf("stg%d" % i) for i in range(NSTG)]
        conv_done = [Buf("conv_done%d" % i) for i in range(NSTG)]
        conv_state = [0]

        def convert_next():
            n_ = conv_state[0]
            if n_ >= 2 * N_ET:
                return
            conv_state[0] += 1
            sl = n_ % NSTG
            e_ = n_ // 2
            if n_ % 2 == 0:
                K.dma(POOL, stg[sl].rearrange("p (k e) -> p k e", k=KC), u_t[e_], W=[Bstg[sl]], own=Bstg[sl])
                K.dma(SP, u_bf[e_], stg[sl].rearrange("p (k e) -> p k e", k=KC), R=[Bstg[sl]], W=[conv_done[sl]], own=Bstg[sl])
            else:
                K.dma(POOL, stg[sl], v_tab[e_ * 128:(e_ + 1) * 128, :], W=[Bstg[sl]], own=Bstg[sl])
                K.dma(SP, v_bf[e_ * 128:(e_ + 1) * 128, :], stg[sl], R=[Bstg[sl]], W=[conv_done[sl]], own=Bstg[sl])

        def qk_proj(w, Bw, gcol, dst, Bdst, itc):
            pbs = []

            def proj(tb):
                pb = itc[0] % 3
                itc[0] += 1
                pbs.append(pb)
                cs = slice(tb * 512, (tb + 1) * 512)
                for kc in range(KC):
                    mm(banks[pb], w[:, kc, :], hT_all[:, kc, cs], kc == 0, kc == KC - 1, [Bw, B_hT], [PB[pb]])

            def fin(tb):
                pb = pbs[tb]
                q = tb % 2
                cs = slice(tb * 512, (tb + 1) * 512)
                act(sqb[q], banks[pb], AF.Square, [PB[pb]], [Bsq[q]])
                mm(banks[3], bonesb, sqb[q], True, True, [B_const, Bsq[q]], [PB[3]])
                act(sdb[q], banks[3], AF.Sqrt, [PB[3], B_misc], [Bsd[q]], bias=epsc, scale=1.0 / 64)
                recip(sdb[q], sdb[q], [Bsd[q]], [Bsd[q]])
                stt(dst[:, cs], banks[pb], gcol, sdb[q], ALU.mult, ALU.mult, [PB[pb], Bsd[q]] + CR, [Bdst])

            for tb in range(NB + 1):
                if tb < NB:
                    proj(tb)
                if tb >= 1:
                    fin(tb - 1)

        itc = [0]
        ipt = 0
        io = 0
        for h in range(8):
            s = h % 2
            load_wblk(wq_s[s], w_in_blk[h], Bwq[s])
            load_wblk(wk_s[s], w_in_blk[8 + h], Bwk[s])
            load_wblk(wv_s[s], w_in_blk[16 + h], Bwv[s])
            qk_proj(wk_s[s], Bwk[s], colt[:, 17:18], KT, B_KT, itc)
            qk_proj(wq_s[s], Bwq[s], gq8, Qall, B_Qall, itc)
            select_own(Qown.rearrange("p (n c) -> p n c", c=128),
                       Qall.rearrange("p (n two c) -> p n two c", two=2, c=128), B_Qall, B_Qown)
            for g4 in range(NT // 4):
                q = 4 + (g4 % 2)
                for tl in range(4):
                    t = g4 * 4 + tl
                    for kc in range(KC):
                        mm(banks[q][:, tl * 128:(tl + 1) * 128], hT_all[:, kc, t * 128:(t + 1) * 128], wv_s[s][:, kc, :],
                           kc == 0, kc == KC - 1, [B_hT, Bwv[s]], [PB[q]])
                cp(ACT if g4 % 2 == 0 else DVE, Vh[:, g4 * 4:g4 * 4 + 4, 0:128], banks[q].rearrange("p (a b) -> p a b", a=4),
                   [PB[q]], [B_Vh])
            items = []
            for i in range(NO):
                groups = []
                full = list(range(2 * i))
                for a in range(0, len(full), 4):
                    groups.append((full[a:a + 4], False))
                groups.append(([2 * i, 2 * i + 1], True))
                for gi_, (tiles, diag) in enumerate(groups):
                    for c in range(2):
                        items.append((i, c, tiles, diag, gi_ == 0, gi_ == len(groups) - 1))

            OBK = [5, 6, 7, 3]

            def emit_S(k):
                i, c, tiles, diag, fg, lg = items[k]
                sbank = k % 3
                for idx, jt in enumerate(tiles):
                    mm(banks[sbank][:, idx * 128:(idx + 1) * 128],
                       KT[c * 64:(c + 1) * 64, jt * 128:(jt + 1) * 128],
                       Qown[c * 64:(c + 1) * 64, i * 128:(i + 1) * 128], True, True, [B_KT, B_Qown], [PB[sbank]])

            def emit_rest(k):
                i, c, tiles, diag, fg, lg = items[k]
                sbank = k % 3
                pt = k % NPT
                n = len(tiles)
                ob_ = OBK[(i % 2) * 2 + c]
                act(pT[pt][:, 0:n * 128], banks[sbank][:, 0:n * 128], AF.Exp, [PB[sbank]], [BpT[pt]])
                if diag:
                    tt(DVE, pT[pt][:, 0:256], pT[pt][:, 0:256], maskb, ALU.mult, [BpT[pt], B_const], [BpT[pt]])
                for idx, jt in enumerate(tiles):
                    mm(banks[ob_][:, 0:130], pT[pt][:, idx * 128:(idx + 1) * 128], Vh[:, jt, :],
                       fg and idx == 0, lg and idx == n - 1, [BpT[pt], B_Vh], [PB[ob_]])
                return lg and c == 1

            def epilogue(i):
                oi = i % 2
                b0, b1 = OBK[oi * 2], OBK[oi * 2 + 1]
                st_ = ast[oi]
                recip(st_[:, 0:1], banks[b0][:, 128:129], [PB[b0]], [Bast[oi]])
                ts(DVE, o0[oi], banks[b0][:, 0:128], st_[:, 0:1], None, ALU.mult, None, [PB[b0], Bast[oi]], [Bo0[oi]])
                recip(st_[:, 1:2], banks[b1][:, 128:129], [PB[b1]], [Bast[oi]])
                tt(DVE, st_[:, 1:2], st_[:, 1:2], nlam, ALU.mult, [Bast[oi], B_misc], [Bast[oi]])
                stt(dif[oi], banks[b1][:, 0:128], st_[:, 1:2], o0[oi], ALU.mult, ALU.add,
                    [PB[b1], Bast[oi], Bo0[oi]], [Bdif[oi]])
                K.op(DVE, lambda: nc.vector.scalar_tensor_tensor(out=ajunk, in0=dif[oi], scalar=1.0, in1=dif[oi], op0=ALU.mult, op1=ALU.mult,
                                                                 accum_out=st_[:, 2:3]), [Bdif[oi]], [B_aj, Bast[oi]])
                ts(DVE, st_[:, 3:4], st_[:, 2:3], 1.0 / 128, EPS, ALU.mult, ALU.add, [Bast[oi]], [Bast[oi]])
                K.op(POOL, lambda: nc.gpsimd.tensor_tensor(out=st_[:, 4:5], in0=st_[:, 3:4], in1=mhalf, op=ALU.pow),
                     [Bast[oi], B_misc], [Bast[oi]])
                stt(dif[oi], dif[oi], st_[:, 4:5], sgrep, ALU.mult, ALU.mult, [Bdif[oi], Bast[oi], B_const], [Bdif[oi]])
                tr(banks[4][:, 0:128], dif[oi], [Bdif[oi]], [PB[4]])
                cp(DVE, attnT[:, h, i * 128:(i + 1) * 128], banks[4][:, 0:128], [PB[4]], [B_attnT])

            LA = 2
            DEF = 3
            pending = []
            for k in range(len(items) + LA):
                if k < len(items):
                    emit_S(k)
                if k >= LA:
                    if emit_rest(k - LA):
                        pending.append([DEF + 1, items[k - LA][0]])
                        convert_next()
                        convert_next()
                    for p_ in pending:
                        p_[0] -= 1
                    while pending and pending[0][0] <= 0:
                        epilogue(pending.pop(0)[1])
            for p_ in pending:
                epilogue(p_[1])
        while conv_state[0] < 2 * N_ET:
            convert_next()
        K.barrier()

        db = Bump(RD)
        wpat = db.take(BF16, [128, 8, 8, 128])
        B_wpa = Buf("wpa")
        for oc in range(8):
            K.dma(POOL, wpat[:, oc, :, :], wpa[oc], W=[B_wpa], own=B_wpa)
        eb = Bump(RE)
        hTo = [eb.take(BF16, [128, KC, 512]) for _ in range(2)]
        BhTo = [Buf("hTo0"), Buf("hTo1")]
        wga = [eb.take(BF16, [128, KC, 128]) for _ in range(2)]
        Bwga = [Buf("wga0"), Buf("wga1")]
        gsg = [eb.take(F32, [128, 512]) for _ in range(2)]
        Bgsg = [Buf("gsa0"), Buf("gsa1")]
        tmpm = [eb.take(F32, [128, 512]) for _ in range(2)]
        Btmpm = [Buf("tmpm0"), Buf("tmpm1")]
        def p4_a(n):
            ob, oc = divmod(n, 8)
            hs_ = ob % 2
            q = n % 2
            if oc == 0:
                for kc in range(KC):
                    select_own(hTo[hs_][:, kc, :].rearrange("p (n c) -> p n c", c=128),
                               hT_all[:, kc, ob * 1024:(ob + 1) * 1024].rearrange("p (n two c) -> p n two c", two=2, c=128),
                               B_hT, BhTo[hs_])
            cs = slice(ob * 512, (ob + 1) * 512)
            load_wblk(wga[q], w_in_blk[44 + oc], Bwga[q])
            for h in range(8):
                mm(banks[q], wpat[:, oc, h, :], attnT[:, h, cs], h == 0, h == 7, [B_wpa, B_attnT], [PB[q]])
            for kc in range(KC):
                mm(banks[2 + q], wga[q][:, kc, :], hTo[hs_][:, kc, :], kc == 0, kc == KC - 1, [Bwga[q], BhTo[hs_]], [PB[2 + q]])

        def p4_b(n):
            ob, oc = divmod(n, 8)
            q = n % 2
            cs = slice(ob * 512, (ob + 1) * 512)
            act(gsg[q], banks[2 + q], AF.Sigmoid, [PB[2 + q], B_const], [Bgsg[q]], bias=colt[:, oc:oc + 1])
            tt(DVE, tmpm[q], banks[q], gsg[q], ALU.mult, [PB[q], Bgsg[q]], [Btmpm[q]])
            tt(POOL, GR[:, oc, cs], GR[:, oc, cs], tmpm[q], ALU.add, [B_GR, Btmpm[q]], [B_GR])

        for n in range(NOB * 8 + 1):
            if n < NOB * 8:
                p4_a(n)
            if n >= 1:
                p4_b(n - 1)
        K.barrier()

        x2 = _view(RA, 0, F32, [128, NO, D])
        B_x2 = [Buf("x2_%d" % i) for i in range(NO)]
        db = Bump(RD)
        woutt = db.take(BF16, [128, KC, D])
        B_wout = Buf("wout")
        for kc in range(KC):
            K.dma(POOL, woutt[:, kc, :], wout[:, kc, :], W=[B_wout], own=B_wout)
        eb = Bump(RE)
        xo = [eb.take(F32, [128, D]) for _ in range(2)]
        Bxo = [Buf("xo0"), Buf("xo1")]
        for i in range(NO):
            s = i % 2
            K.dma(SP, xo[s], x_own[i * 128:(i + 1) * 128, :], W=[Bxo[s]], own=Bxo[s])
            for nb in range(2):
                q = 2 * s + nb
                for kc in range(KC):
                    mm(banks[q], GR[:, kc, i * 128:(i + 1) * 128], woutt[:, kc, nb * 512:(nb + 1) * 512], kc == 0, kc == KC - 1,
                       [B_GR, B_wout], [PB[q]])
                tt(DVE, x2[:, i, nb * 512:(nb + 1) * 512], banks[q], xo[s][:, nb * 512:(nb + 1) * 512], ALU.add,
                   [PB[q], Bxo[s]], [B_x2[i]])
        B_dbg = Buf("dbgst")
        if debug:
            for i in range(NO):
                K.dma(SP, dbg[i * 128:(i + 1) * 128, :], x2[:, i, :], R=[B_x2[i]], own=B_dbg)
        K.barrier()

        U32 = mybir.dt.uint32
        TB = 256
        NBLK = SO // TB
        CH = 8
        GT = _view(RBC, 0, BF16, [128, TB, 128])
        B_GT = Buf("GT")
        db = Bump(RD)
        h2Ts = [db.take(BF16, [128, KC, TB]) for _ in range(2)]
        B_h2Ts = [Buf("h2T0"), Buf("h2T1")]
        NU, NV = 4, 6
        Ut = [db.take(BF16, [128, KC, 128]) for _ in range(NU)]
        BUt = [Buf("Ut%d" % i) for i in range(NU)]
        Vt = [db.take(BF16, [128, D]) for _ in range(NV)]
        BVt = [Buf("Vt%d" % i) for i in range(NV)]
        GwT = [db.take(BF16, [128, EG, TB]) for _ in range(2)]
        BGw = [Buf("Gw0"), Buf("Gw1")]
        BGwR = [Buf("GwR%d" % i) for i in range(4)]
        geb = [db.take(BF16, [128, TB]) for _ in range(2)]
        Bge = [Buf("ge0"), Buf("ge1")]
        idxTs = [db.take(BF16, [128, 2, 128]) for _ in range(2)]
        B_idxTs = [Buf("idxT0"), Buf("idxT1")]
        ssb = [db.take(F32, [128, 256]) for _ in range(2)]
        Bssb = [Buf("ssb0"), Buf("ssb1")]
        iotab = banks[3][:, 256:384]
        B_iob = PB[3]
        cp(DVE, iotab, iotat, [B_const], [PB[3]])
        OH1 = [Vt[k].rearrange("p (c i) -> p c i", c=CH) for k in (0, 1)]
        OH2 = [Vt[k].rearrange("p (c i) -> p c i", c=CH) for k in (2, 3)]
        WF = [Vt[k].rearrange("p (c h r) -> p c h r", c=CH, h=8) for k in (4, 5)]
        BOH1, BOH2, BWF = [BVt[0], BVt[1]], [BVt[2], BVt[3]], [BVt[4], BVt[5]]
        Mt = [Ut[k][:, 0:4, :] for k in (0, 1)]
        BMt = [BUt[0], BUt[1]]
        eb = Bump(RE)
        keyst = eb.take(BF16, [128, 16, 128])
        B_keys = Buf("keys")
        K.dma(POOL, keyst, keysT[:, :, :], W=[B_keys], own=B_keys)
        wqs = [eb.take(BF16, [128, KC, 128]) for _ in range(2)]
        Bwqs = [Buf("wqs0"), Buf("wqs1")]
        qTb = [eb.take(BF16, [128, TB]) for _ in range(2)]
        BqTb = [Buf("qTb0"), Buf("qTb1")]
        pxn = eb.take(F32, [128, D])
        pstt = eb.take(F32, [128, 8])
        B_pscr = Buf("pscr")
        topv = [eb.take(F32, [128, 16, 16]) for _ in range(2)]
        idxu = [eb.take(U32, [128, 16, 16]) for _ in range(2)]
        B_top = [Buf("topv0"), Buf("topv1")]
        wk1 = eb.take(F32, [128, 128])
        B_wk1 = Buf("wk1")
        cand = eb.take(F32, [128, 256])
        cwk = eb.take(F32, [128, 256])
        B_cand, B_cwk = Buf("cand"), Buf("cwk")
        ctop = eb.take(F32, [128, 8, 16])
        B_ctop = Buf("ctop")
        pst = eb.take(F32, [128, 64])
        B_pst = Buf("pst")
        ejunk = eb.take(F32, [128, 16])
        B_ej = Buf("ejunk")
        Fw = [eb.take(F32, [128, 256]) for _ in range(2)]
        BFw = [Buf("Fw0"), Buf("Fw1")]
        Wr = db.take(BF16, [128, 16, 128])
        B_Wr = Buf("Wr")
        idxf = eb.take(F32, [128, 2, 128])
        B_idxf = Buf("idxf")
        WTs = [eb.take(BF16, [128, 16, 128]) for _ in range(2)]
        B_WTs = [Buf("WT0"), Buf("WT1")]
        B_out = Buf("outst")
        g2c = colt[:, 20:28]

        def load_U(et):
            sl, e_ = et % NU, et % N_ET
            K.dma(SP, Ut[sl], u_bf[e_], R=conv_done, W=[BUt[sl]], own=BUt[sl])

        def load_V(et):
            sl, e_ = et % NV, et % N_ET
            K.dma(SP, Vt[sl], v_bf[e_ * 128:(e_ + 1) * 128, :], R=conv_done, W=[BVt[sl]], own=BVt[sl])

        def bf(bank):
            return banks[bank].bitcast(BF16)

        PB3q, PB3s = PB[3], PB[2]

        def prep(blk):
            h2T, B_h2T = h2Ts[blk % 2], B_h2Ts[blk % 2]
            for tl in range(2):
                i = blk * 2 + tl
                xt = x2[:, i, :]
                act(pxn, xt, AF.Square, [B_x2[i]], [B_pscr], accum=pstt[:, 0:1])
                ts(DVE, pstt[:, 1:2], pstt[:, 0:1], 1.0 / D, EPS, ALU.mult, ALU.add, [B_pscr], [B_pscr])
                act(pstt[:, 1:2], pstt[:, 1:2], AF.Sqrt, [B_pscr], [B_pscr])
                recip(pstt[:, 2:3], pstt[:, 1:2], [B_pscr], [B_pscr])
                ts(DVE, pxn, xt, pstt[:, 2:3], None, ALU.mult, None, [B_x2[i], B_pscr], [B_pscr])
                yield
                for half in range(2):
                    for q in range(4):
                        kc = half * 4 + q
                        tr(banks[2][:, q * 128:(q + 1) * 128], pxn[:, kc * 128:(kc + 1) * 128], [B_pscr], [PB[2]])
                    for q in range(4):
                        kc = half * 4 + q
                        dst = h2T[:, kc, tl * 128:(tl + 1) * 128]
                        src = banks[2][:, q * 128:(q + 1) * 128]
                        if q % 2:
                            ts(DVE, dst, src, g2c[:, kc:kc + 1], None, ALU.mult, None, [PB[2], B_const], [B_h2T])
                        else:
                            act(dst, src, AF.Copy, [PB[2], B_const], [B_h2T], scale=g2c[:, kc:kc + 1])
                    yield
            K.dma(POOL, wqs[0], wq[0], W=[Bwqs[0]], own=Bwqs[0])

            def q_part(hc):
                q = hc % 2
                if hc + 1 < 16:
                    K.dma(POOL, wqs[1 - q], wq[hc + 1], W=[Bwqs[1 - q]], own=Bwqs[1 - q])
                for kc in range(KC):
                    mm(banks[3][:, 0:TB], wqs[q][:, kc, :], h2T[:, kc, :], kc == 0, kc == KC - 1, [Bwqs[q], B_h2T], [PB3q])
                cp(ACT, qTb[q], banks[3][:, 0:TB], [PB3q], [BqTb[q]])

            def s_part(hc, tl):
                q = hc % 2
                if tl == 0:
                    for t2 in range(2):
                        mm(banks[2][:, t2 * 128:(t2 + 1) * 128], qTb[q][:, t2 * 128:(t2 + 1) * 128], keyst[:, hc, :], True, True,
                           [BqTb[q], B_keys], [PB3s])
                    cp(ACT, ssb[q], banks[2][:, 0:256], [PB3s], [Bssb[q]])
                sv = ssb[q][:, tl * 128:(tl + 1) * 128]
                tv, iu, Bt = topv[tl], idxu[tl], B_top[tl]
                K.op(DVE, lambda: nc.vector.max(out=tv[:, hc, 0:8], in_=sv), [Bssb[q]], [Bt])
                K.op(DVE, lambda: nc.vector.max_index(out=iu[:, hc, 0:8], in_max=tv[:, hc, 0:8], in_values=sv), [Bssb[q], Bt], [Bt])
                K.op(DVE, lambda: nc.vector.match_replace(out=wk1, in_to_replace=tv[:, hc, 0:8], in_values=sv, imm_value=-1e30),
                     [Bssb[q], Bt], [B_wk1])
                K.op(DVE, lambda: nc.vector.max(out=tv[:, hc, 8:16], in_=wk1), [B_wk1], [Bt])
                K.op(DVE, lambda: nc.vector.max_index(out=iu[:, hc, 8:16], in_max=tv[:, hc, 8:16], in_values=wk1), [B_wk1, Bt], [Bt])

            for hc in range(17):
                if hc < 16:
                    q_part(hc)
                    yield
                if hc >= 1:
                    s_part(hc - 1, 0)
                    yield
                    s_part(hc - 1, 1)
                    yield
            for tl in range(2):
                tv, iu, Bt = topv[tl], idxu[tl], B_top[tl]
                idxT, B_idxT, WT, B_WT = idxTs[tl], B_idxTs[tl], WTs[tl], B_WTs[tl]
                cand3 = cand.rearrange("p (a b) -> p a b", a=16)

                def mk_cand(h):
                    in0 = tv[:, 2 * h, :].unsqueeze(2).to_broadcast([128, 16, 16])
                    in1 = tv[:, 2 * h + 1, :].unsqueeze(1).to_broadcast([128, 16, 16])
                    tt(DVE, cand3, in0, in1, ALU.add, [Bt], [B_cand])
                for h in range(8):
                    mk_cand(h)
                    K.op(DVE, lambda h=h: nc.vector.max(out=ctop[:, h, 0:8], in_=cand), [B_cand], [B_ctop])
                    K.op(DVE, lambda h=h: nc.vector.match_replace(out=cwk, in_to_replace=ctop[:, h, 0:8], in_values=cand,
                                                                  imm_value=-1e30), [B_cand, B_ctop], [B_cwk])
                    K.op(DVE, lambda h=h: nc.vector.max(out=ctop[:, h, 8:16], in_=cwk), [B_cwk], [B_ctop])
                    yield
                ts(DVE, pst[:, 0:8], ctop[:, :, 0], -1.0, None, ALU.mult, None, [B_ctop], [B_pst])
                for h in range(8):
                    act(ejunk, ctop[:, h, :], AF.Exp, [B_ctop, B_pst], [B_ej, B_pst], bias=pst[:, h:h + 1], accum=pst[:, 8 + h:9 + h])
                act(pst[:, 16:24], pst[:, 8:16], AF.Ln, [B_pst], [B_pst])
                tt(DVE, pst[:, 24:32], pst[:, 0:8], pst[:, 16:24], ALU.subtract, [B_pst], [B_pst])
                ts(DVE, pst[:, 32:40], ctop[:, :, 15], -4e-6, None, ALU.add, None, [B_ctop, B_pst], [B_pst])
                tv4 = tv.rearrange("p (h c) r -> p h c r", c=2)
                E1 = cwk[:, 0:128].rearrange("p (h r) -> p h r", h=8)
                E2 = cwk[:, 128:256].rearrange("p (h r) -> p h r", h=8)
                act(E1, tv4[:, :, 0, :], AF.Exp, [Bt, B_cwk], [B_cwk])
                act(E2, tv4[:, :, 1, :], AF.Exp, [Bt, B_cwk], [B_cwk])
                act(pst[:, 40:48], pst[:, 24:32], AF.Exp, [B_pst], [B_pst])
                tt(DVE, E1, E1, pst[:, 40:48].unsqueeze(2).to_broadcast([128, 8, 16]), ALU.mult, [B_cwk, B_pst], [B_cwk])
                yield
                for h in range(8):
                    fq = h % 2
                    mk_cand(h)
                    fw3 = Fw[fq].rearrange("p (a b) -> p a b", a=16)
                    tt(DVE, fw3, E1[:, h, :].unsqueeze(2).to_broadcast([128, 16, 16]),
                       E2[:, h, :].unsqueeze(1).to_broadcast([128, 16, 16]), ALU.mult, [B_cwk], [BFw[fq]])
                    wout_v = Wr[:, :, h * 16:(h + 1) * 16].rearrange("p r2 r1 -> p r1 r2")
                    stt(wout_v, cand3, pst[:, 32 + h:33 + h], fw3, ALU.is_ge, ALU.mult,
                        [B_cand, B_pst, BFw[fq]], [B_Wr])
                    yield
                iu4 = iu.rearrange("p (h c) r -> p h c r", c=2)
                for c in range(2):
                    cp(DVE, idxf[:, c, :].rearrange("p (h r) -> p h r", h=8), iu4[:, :, c, :], [Bt], [B_idxf])
                for c in range(2):
                    tr(banks[2][:, c * 128:(c + 1) * 128], idxf[:, c, :], [B_idxf], [PB[2]])
                cp(ACT, idxT.rearrange("p c t -> p (c t)"), banks[2][:, 0:256], [PB[2]], [B_idxT])
                yield
                for g8 in range(2):
                    for r8 in range(8):
                        r2 = g8 * 8 + r8
                        K.op(PE, lambda r2=r2, r8=r8: nc.tensor.transpose(bf(2)[:, r8 * 128:(r8 + 1) * 128], Wr[:, r2, :], identb),
                             [B_Wr, B_const], [PB[2]])
                    cp(ACT if g8 == 0 else DVE, WT[:, g8 * 8:(g8 + 1) * 8, :].rearrange("p r t -> p (r t)"), bf(2), [PB[2]], [B_WT])
                    yield

        def gbuild(blk):
            for tl in range(2):
                idxT, B_idxT, WT, B_WT = idxTs[tl], B_idxTs[tl], WTs[tl], B_WTs[tl]
                ngrp = 128 // 4

                def build_chunk(ch):
                    b_ = ch % 2
                    t0 = ch * CH
                    io_ = iotab.unsqueeze(1).to_broadcast([128, CH, 128])
                    tt(DVE, OH1[b_], io_, idxT[:, 0, t0:t0 + CH].unsqueeze(2).to_broadcast([128, CH, 128]), ALU.is_equal,
                       [B_iob, B_idxT], [BOH1[b_]])
                    tt(DVE, OH2[b_], io_, idxT[:, 1, t0:t0 + CH].unsqueeze(2).to_broadcast([128, CH, 128]), ALU.is_equal,
                       [B_iob, B_idxT], [BOH2[b_]])
                    w_in0 = WT[:, :, t0:t0 + CH].rearrange("p r t -> p t r").unsqueeze(2).to_broadcast([128, CH, 8, 16])
                    h_in1 = hmb.unsqueeze(1).unsqueeze(3).to_broadcast([128, CH, 8, 16])
                    tt(POOL, WF[b_], w_in0, h_in1, ALU.mult, [B_WT, B_const], [BWF[b_]])

                def emit1(g):
                    ch = (g * 4) // CH
                    if (g * 4) % CH == 0:
                        build_chunk(ch)
                    b_ = ch % 2
                    mq = g % 2
                    for k in range(4):
                        tk = (g * 4) % CH + k
                        mm(banks[mq][:, k * 128:(k + 1) * 128], WF[b_][:, tk, :, :].rearrange("p h r -> p (h r)"), OH1[b_][:, tk, :],
                           True, True, [BWF[b_], BOH1[b_]], [PB[mq]])
                    cp(ACT, Mt[mq].rearrange("p k i -> p (k i)"), banks[mq], [PB[mq]], [BMt[mq]])

                def emit2(g):
                    ch = (g * 4) // CH
                    b_ = ch % 2
                    mq = g % 2
                    for k in range(4):
                        tk = (g * 4) % CH + k
                        mm(banks[4 + mq][:, k * 128:(k + 1) * 128], OH2[b_][:, tk, :], Mt[mq][:, k, :], True, True,
                           [BOH2[b_], BMt[mq]], [PB[4 + mq]])
                    tg = tl * 128 + g * 4
                    cp(ACT, GT[:, tg:tg + 4, :].rearrange("p t i -> p (t i)"), banks[4 + mq], [PB[4 + mq]], [B_GT])

                for g in range(ngrp + 1):
                    if g < ngrp:
                        emit1(g)
                    if g >= 1:
                        emit2(g - 1)

        etc = 0
        for _ in prep(0):
            pass
        for blk in range(NBLK):
            h2T, B_h2T = h2Ts[blk % 2], B_h2Ts[blk % 2]
            gbuild(blk)
            nxt = prep(blk + 1) if blk + 1 < NBLK else iter(())
            load_U(etc)
            load_V(etc)
            load_U(etc + 1)
            load_V(etc + 1)
            load_U(etc + 2)

            def emit_A(g_et):
                us = g_et % NU
                aq = g_et % 2
                for kc in range(KC):
                    mm(banks[aq][:, 0:TB], Ut[us][:, kc, :], h2T[:, kc, :], kc == 0, kc == KC - 1, [BUt[us], B_h2T], [PB[aq]])

            emit_A(etc)
            emit_A(etc + 1)
            for et in range(N_ET):
                ge_ = etc
                if et + 3 < N_ET:
                    load_U(etc + 3)
                if et + 2 < N_ET:
                    load_V(etc + 2)
                aq = ge_ % 2
                if et % 2 == 0:
                    act(geb[0], banks[0][:, 0:TB], AF.Gelu_apprx_tanh, [PB[0]], [Bge[0]])
                    act(geb[1], banks[1][:, 0:TB], AF.Gelu_apprx_tanh, [PB[1]], [Bge[1]])
                    if et + 2 < N_ET:
                        emit_A(etc + 2)
                        emit_A(etc + 3)
                gs = ge_ % 4
                gw_t = GwT[gs // 2][:, (gs % 2) * 2:(gs % 2) * 2 + 1, :].rearrange("p a t -> p (a t)")
                tt(POOL, gw_t, GT[:, :, et], geb[aq], ALU.mult, [B_GT, Bge[aq]], [BGwR[gs]])
                vs = ge_ % NV
                for tl in range(2):
                    for nb in range(2):
                        oq = 4 + 2 * tl + nb
                        mm(banks[oq], gw_t[:, tl * 128:(tl + 1) * 128], Vt[vs][:, nb * 512:(nb + 1) * 512],
                           et == 0, et == N_ET - 1, [BGwR[gs], BVt[vs]], [PB[oq]])
                etc += 1
                next(nxt, None)
            for tl in range(2):
                i = blk * 2 + tl
                for nb in range(2):
                    oq = 4 + 2 * tl + nb
                    tt(DVE, x2[:, i, nb * 512:(nb + 1) * 512], banks[oq], x2[:, i, nb * 512:(nb + 1) * 512], ALU.add,
                       [PB[oq], B_x2[i]], [B_x2[i]])
            for _ in nxt:
                pass
            for tl in range(2):
                i = blk * 2 + tl
                K.dma(SP, out_own[i * 128:(i + 1) * 128, :], x2[:, i, :], R=[B_x2[i]], own=B_out)
        K.barrier()
    return nc


def _prep_shared(inp):
    f = np.float32
    w_in = np.asarray(inp["w_in"][0], f)
    sh = {}
    sh["w_in_blk"] = np.ascontiguousarray(w_in.reshape(KC, 128, 60, 128).transpose(2, 1, 0, 3))
    sh["g1"] = np.ascontiguousarray(inp["norm1_g"][0], f)
    sh["g2"] = np.ascontiguousarray(inp["norm2_g"][0], f)
    sh["lamv"] = np.ascontiguousarray(np.concatenate([inp["lambda_q1"][0], inp["lambda_k1"][0],
                                                      inp["lambda_q2"][0], inp["lambda_k2"][0]]), f)
    sh["sublng"] = np.ascontiguousarray(inp["subln_g"][0], f)
    rv = np.zeros((128, 10, 8), f)
    cw = np.asarray(inp["conv_w"][0], f)
    for tap in range(4):
        rv[:, :, tap] = cw[tap].reshape(10, 128).T
    rv[:, :, 4] = np.asarray(inp["conv_b"][0], f).reshape(10, 128).T
    rv[:, :, 5] = np.asarray(inp["b_rg_a"][0], f).reshape(10, 128).T
    rv[:, :, 6] = np.asarray(inp["b_rg_x"][0], f).reshape(10, 128).T
    rv[:, :, 7] = np.asarray(inp["rg_lambda"][0], f).reshape(10, 128).T
    sh["rnnvec"] = rv
    wbd = np.zeros((128, 10, 2, 128), f)
    wa = np.asarray(inp["w_rg_a"][0], f)
    wx = np.asarray(inp["w_rg_x"][0], f)
    for j in range(10):
        for half in range(2):
            sl = slice(half * 64, (half + 1) * 64)
            wbd[sl, j, 0, sl] = wa[2 * j + half]
            wbd[sl, j, 1, sl] = wx[2 * j + half]
    sh["wbd"] = wbd
    sh["wpa"] = np.ascontiguousarray(np.asarray(inp["w_br_attn"][0], f).reshape(8, 128, 8, 128).transpose(2, 1, 0, 3))
    sh["wpr"] = np.ascontiguousarray(np.asarray(inp["w_br_rnn"][0], f).reshape(10, 128, 8, 128).transpose(2, 1, 0, 3))
    sh["wout"] = np.ascontiguousarray(np.asarray(inp["w_out"][0], f).reshape(KC, 128, D).transpose(1, 0, 2))
    sh["wq"] = np.ascontiguousarray(np.asarray(inp["w_peer_q"][0], f).reshape(KC, 128, 16, 128).transpose(2, 1, 0, 3))
    sk = np.asarray(inp["peer_sub_keys"][0], f)
    sh["keysT"] = np.ascontiguousarray(sk.reshape(16, 128, 128).transpose(2, 0, 1))
    pu = np.asarray(inp["peer_u"][0], f)
    sh["u_t"] = np.ascontiguousarray(pu.reshape(N_ET, 128, KC, 128).transpose(0, 3, 2, 1))
    sh["v_tab"] = np.ascontiguousarray(inp["peer_v"][0], f)
    sh["ident"] = np.eye(128, dtype=f)
    bo = np.zeros((128, 128), f)
    bo[:64, :64] = 1
    bo[64:, 64:] = 1
    sh["bones"] = bo
    sh["iota"] = np.ascontiguousarray(np.broadcast_to(np.arange(128, dtype=f)[None, :], (128, 128)))
    hm = np.zeros((128, 8), f)
    hm[np.arange(128), np.arange(128) // 16] = 1.0
    sh["hm"] = hm
    return sh


def _prep_core(inp, sh, b, p, NT):
    f = np.float32
    m = dict(sh)
    x = np.asarray(inp["x"][b], f)[:NT * 128]
    m["x_all"] = np.ascontiguousarray(x)
    m["x_own"] = np.ascontiguousarray(x.reshape(NT // 2, 2, 128, D)[:, p].reshape(-1, D))
    cols = np.zeros((128, 32), f)
    cols[:, 0:16] = np.asarray(inp["b_gate"][0], f).reshape(16, 128).T
    cols[:, 16] = np.tile(np.asarray(inp["q_norm_g"][0], f), 2)
    cols[:, 17] = np.tile(np.asarray(inp["k_norm_g"][0], f), 2)
    cols[:, 18] = 1.0 if p == 0 else 0.0
    cols[:, 19] = 0.0 if p == 0 else 1.0
    cols[:, 20:28] = np.asarray(inp["norm2_g"][0], f).reshape(8, 128).T
    m["cols"] = cols
    k = np.arange(128)[:, None] // 64
    q = np.arange(128)[None, :] // 64
    dg = (k <= q).astype(f)
    if p == 0:
        m["mask2"] = np.concatenate([dg, np.zeros((128, 128), f)], axis=1)
    else:
        m["mask2"] = np.concatenate([np.ones((128, 128), f), dg], axis=1)
    return m


def run(inputs, NT=32, debug=False, n_batch=4):
    sh = _prep_shared(inputs)
    maps = []
    for c in range(2 * n_batch):
        maps.append(_prep_core(inputs, sh, c // 2, c % 2, NT))
    nc = build_nc(NT, debug=debug)
    res = run_bass_kernel_spmd(nc, maps, core_ids=list(range(2 * n_batch)))
    out = np.zeros((n_batch, NT * 128, D), np.float32)
    dbg = np.zeros_like(out) if debug else None
    for c in range(2 * n_batch):
        b, p = c // 2, c % 2
        r = res.results[c]
        out[b].reshape(NT // 2, 2, 128, D)[:, p] = np.asarray(r["out_own"]).reshape(NT // 2, 128, D)
        if debug:
            dbg[b].reshape(NT // 2, 2, 128, D)[:, p] = np.asarray(r["dbg_x2"]).reshape(NT // 2, 128, D)
    return (out, dbg) if debug else out


def kernel(**inputs):
    return run(inputs, NT=32, debug=False, n_batch=4)
```
